# Optimizing a Trainium2 kernel written in Bass

```python
import math
import jax
import jax.numpy as jnp
from jax import lax
import numpy as np

D_MODEL = 1024
BATCH = 4
SEQ = 4096
DEPTH = 4

CTX_LEN = 256
GRID_W = 64

A_HEADS = 4
A_HEAD_DIM = 64
A_WIDTH = A_HEADS * 2 * A_HEAD_DIM

G_HEADS = 4
G_KEY_DIM = 128
G_VAL_DIM = 128
G_QK_WIDTH = G_HEADS * G_KEY_DIM
G_V_WIDTH = G_HEADS * G_VAL_DIM
CONV_K = 5
CHUNK = 64

N_EXPERTS = 16
N_GROUPS = 4
EXPERTS_PER_GROUP = N_EXPERTS // N_GROUPS
TOP_K = 2
GROUP_SCORE_TOPK = 2
D_EXPERT = 512
MOE_BLOCK = 256

Q_BLOCK = 128
ROPE_BASE = 10000.0
EPS = 1e-6

IN_SIZES = (A_WIDTH, A_WIDTH, A_WIDTH, 2 * G_QK_WIDTH + G_V_WIDTH, G_V_WIDTH, 2 * G_HEADS, 2 * G_HEADS, 2 * D_MODEL)
IN_OFFSETS = tuple(sum(IN_SIZES[:i + 1]) for i in range(len(IN_SIZES) - 1))
N_IN = sum(IN_SIZES)

kernel_name = 'hybrid_diffattn_gdn_moe_dit'


def layer_norm(x, g, b):
    xf = x.astype(jnp.float32)
    mu = jnp.mean(xf, axis=-1, keepdims=True)
    var = jnp.mean(jnp.square(xf - mu), axis=-1, keepdims=True)
    return ((xf - mu) * lax.rsqrt(var + EPS)).astype(x.dtype) * g + b


def rms_norm(x, g):
    xf = x.astype(jnp.float32)
    return (xf * lax.rsqrt(jnp.mean(jnp.square(xf), axis=-1, keepdims=True) + EPS)).astype(x.dtype) * g


def l2_normalize(x):
    xf = x.astype(jnp.float32)
    return (xf * lax.rsqrt(jnp.sum(jnp.square(xf), axis=-1, keepdims=True) + EPS)).astype(x.dtype)


def modulate(x, shift, scale):
    return x * (1.0 + scale) + shift


def split_in(p):
    return jnp.split(p, IN_OFFSETS, axis=-1)


def axial_rope(n_tokens, dim):
    rows = n_tokens // GRID_W
    row = jnp.repeat(jnp.arange(rows, dtype=jnp.float32), GRID_W)
    col = jnp.tile(jnp.arange(GRID_W, dtype=jnp.float32), rows)
    n_freq = dim // 4
    inv = ROPE_BASE ** (-jnp.arange(n_freq, dtype=jnp.float32) / n_freq)
    ang = jnp.stack([row[:, None] * inv, col[:, None] * inv], axis=1)
    return jnp.cos(ang), jnp.sin(ang)


def rope_2d(x, cos, sin):
    xs = x.reshape(x.shape[:-1] + (2, 2, -1))
    c = cos[:, None, None, :, :]
    s = sin[:, None, None, :, :]
    x1 = xs[..., 0, :]
    x2 = xs[..., 1, :]
    out = jnp.stack([x1 * c - x2 * s, x2 * c + x1 * s], axis=-2)
    return out.reshape(x.shape)


def diff_attend(q, k, v, lam):
    s = jnp.einsum('bqhnd,bkhnd->bnhqk', q, k).astype(jnp.float32) * (A_HEAD_DIM ** -0.5)
    p = jax.nn.softmax(s, axis=-1)
    p = p[:, 0] - lam * p[:, 1]
    return jnp.einsum('bhqk,bkhe->bqhe', p.astype(v.dtype), v)


def short_conv(x, w):
    return lax.conv_general_dilated(x, w[:, None, :], window_strides=(1,),
                                    padding=[(CONV_K // 2, CONV_K // 2)],
                                    dimension_numbers=('NWC', 'WIO', 'NWC'),
                                    feature_group_count=x.shape[-1])


def gated_delta_rule(q, k, v, g, beta, state):
    B, L, H, DK = q.shape
    DV = v.shape[-1]
    n = L // CHUNK
    f32 = jnp.float32

    def chunk4(t):
        return t.astype(f32).reshape(B, n, CHUNK, H, t.shape[-1]).transpose(1, 0, 3, 2, 4)

    def chunk3(t):
        return t.astype(f32).reshape(B, n, CHUNK, H).transpose(1, 0, 3, 2)

    qc = chunk4(q) * (DK ** -0.5)
    kc = chunk4(k)
    vc = chunk4(v)
    bc = chunk3(beta)
    gcum = jnp.cumsum(chunk3(g), axis=-1)
    idx = jnp.arange(CHUNK)
    causal = idx[:, None] >= idx[None, :]
    strict = idx[:, None] > idx[None, :]
    decay = jnp.exp(jnp.where(causal, gcum[..., :, None] - gcum[..., None, :], -jnp.inf))
    kb = kc * bc[..., None]
    a_mat = jnp.where(strict, jnp.einsum('nbhid,nbhjd->nbhij', kb, kc) * decay, 0.0)
    rhs = jnp.concatenate([vc * bc[..., None], kb * jnp.exp(gcum)[..., None]], axis=-1)
    sol = lax.linalg.triangular_solve(a_mat + jnp.eye(CHUNK, dtype=f32), rhs, left_side=True,
                                      lower=True, unit_diagonal=True)
    u, w = sol[..., :DV], sol[..., DV:]
    qk = jnp.where(causal, jnp.einsum('nbhid,nbhjd->nbhij', qc, kc) * decay, 0.0)
    q_dec = qc * jnp.exp(gcum)[..., None]
    k_dec = kc * jnp.exp(gcum[..., -1:] - gcum)[..., None]
    g_last = jnp.exp(gcum[..., -1])

    def step(s, xs):
        q_d, k_d, u_n, w_n, qk_n, gl = xs
        v_new = u_n - jnp.einsum('bhcd,bhde->bhce', w_n, s)
        o = jnp.einsum('bhcd,bhde->bhce', q_d, s) + jnp.einsum('bhij,bhje->bhie', qk_n, v_new)
        s = s * gl[..., None, None] + jnp.einsum('bhcd,bhce->bhde', k_d, v_new)
        return s, o

    s_final, o = lax.scan(step, state.astype(f32), (q_dec, k_dec, u, w, qk, g_last))
    o = o.transpose(1, 0, 3, 2, 4).reshape(B, L, H, DV)
    return o, s_final


def token_mixer(h_lat, h_ctx, cos, sin, lam_init, ctx_out, w_in, conv_w, lam_q1, lam_k1, lam_q2, lam_k2,
                subln_g, a_log, dt_bias, onorm_g, w_pa, w_pb, w_o):
    B, S, _ = h_lat.shape
    qa_l, ka_l, va_l, qkv_l, z_l, a_l, b_l, gate_l = split_in(h_lat @ w_in)
    qa_c, ka_c, va_c, qkv_c, z_c, a_c, b_c, gate_c = split_in(h_ctx @ w_in)

    def heads_a(t):
        return t.reshape(t.shape[:2] + (A_HEADS, 2, A_HEAD_DIM))

    def heads_v(t):
        return t.reshape(t.shape[:2] + (A_HEADS, 2 * A_HEAD_DIM))

    def subln(o):
        return (rms_norm(o, subln_g) * (1.0 - lam_init)).reshape(o.shape[:2] + (A_WIDTH,))

    lam = (jnp.exp(jnp.sum(lam_q1 * lam_k1).astype(jnp.float32))
           - jnp.exp(jnp.sum(lam_q2 * lam_k2).astype(jnp.float32)) + lam_init)
    qa_l = rope_2d(heads_a(qa_l), cos, sin)
    k_ctx = heads_a(ka_c)
    v_ctx = heads_v(va_c)
    k_all = jnp.concatenate([k_ctx, rope_2d(heads_a(ka_l), cos, sin)], axis=1)
    v_all = jnp.concatenate([v_ctx, heads_v(va_l)], axis=1)
    q_blocks = qa_l.reshape((B, S // Q_BLOCK, Q_BLOCK) + qa_l.shape[2:]).swapaxes(0, 1)
    oa_l = lax.map(lambda qb: diff_attend(qb, k_all, v_all, lam), q_blocks)
    ya_l = subln(oa_l.swapaxes(0, 1).reshape(B, S, A_HEADS, 2 * A_HEAD_DIM))

    def gdn_branch(qkv, z, a, b, states):
        L = qkv.shape[1]
        qkv = jax.nn.silu(short_conv(qkv, conv_w))
        q, k, v = jnp.split(qkv, [G_QK_WIDTH, 2 * G_QK_WIDTH], axis=-1)
        q = l2_normalize(q.reshape(B, L, G_HEADS, G_KEY_DIM))
        k = l2_normalize(k.reshape(B, L, G_HEADS, G_KEY_DIM))
        v = v.reshape(B, L, G_HEADS, G_VAL_DIM)
        g = -jnp.exp(a_log.astype(jnp.float32)) * jax.nn.softplus(
            a.reshape(B, L, 2, G_HEADS).astype(jnp.float32) + dt_bias.astype(jnp.float32))
        beta = jax.nn.sigmoid(b.reshape(B, L, 2, G_HEADS).astype(jnp.float32))
        flip = lambda t: jnp.flip(t, axis=1)
        o_f, s_f = gated_delta_rule(q, k, v, g[:, :, 0], beta[:, :, 0], states[0])
        o_b, s_b = gated_delta_rule(flip(q), flip(k), flip(v), flip(g[:, :, 1]), flip(beta[:, :, 1]), states[1])
        o = (o_f + flip(o_b)).astype(z.dtype)
        y = rms_norm(o, onorm_g) * jax.nn.silu(z.reshape(B, L, G_HEADS, G_VAL_DIM))
        return y.reshape(B, L, G_V_WIDTH), (s_f, s_b)

    zeros = jnp.zeros((B, G_HEADS, G_KEY_DIM, G_VAL_DIM), jnp.float32)
    yb_c, ctx_states = gdn_branch(qkv_c, z_c, a_c, b_c, (zeros, zeros))
    yb_l, _ = gdn_branch(qkv_l, z_l, a_l, b_l, ctx_states)

    def merge(ya, yb, gates):
        g_a, g_b = jnp.split(jax.nn.sigmoid(gates), 2, axis=-1)
        return (g_a * (ya @ w_pa) + g_b * (yb @ w_pb)) @ w_o

    y_lat = merge(ya_l, yb_l, gate_l)
    if not ctx_out:
        return y_lat, None
    ya_c = subln(diff_attend(heads_a(qa_c), k_ctx, v_ctx, lam))
    y_ctx = merge(ya_c, yb_c, gate_c)
    return y_lat, y_ctx


def moe_ffn(h, router_w, router_b, w1, w3, w2):
    T, D = h.shape
    scores = jax.nn.sigmoid((h @ router_w).astype(jnp.float32))
    sel = (scores + router_b.astype(jnp.float32)).reshape(T, N_GROUPS, EXPERTS_PER_GROUP)
    group_score = jnp.sum(lax.top_k(sel, GROUP_SCORE_TOPK)[0], axis=-1)
    best = jnp.argmax(group_score, axis=-1).astype(jnp.int32)
    in_group = jnp.take_along_axis(sel, best[:, None, None], axis=1)[:, 0]
    _, local = lax.top_k(in_group, TOP_K)
    expert_idx = best[:, None] * EXPERTS_PER_GROUP + local
    w = jnp.take_along_axis(scores, expert_idx, axis=1)
    w = w / jnp.sum(w, axis=-1, keepdims=True)

    n_assign = T * TOP_K
    flat_e = expert_idx.reshape(-1)
    order = jnp.argsort(flat_e)
    sorted_e = flat_e[order]
    counts = jnp.bincount(flat_e, length=N_EXPERTS)
    padded = (counts + MOE_BLOCK - 1) // MOE_BLOCK * MOE_BLOCK
    pad_end = jnp.cumsum(padded)
    pad_start = pad_end - padded
    start = jnp.cumsum(counts) - counts
    dest_sorted = pad_start[sorted_e] + jnp.arange(n_assign) - start[sorted_e]
    dest = jnp.zeros_like(dest_sorted).at[order].set(dest_sorted)
    n_blocks = -(-n_assign // MOE_BLOCK) + N_EXPERTS
    tok = jnp.arange(n_assign, dtype=jnp.int32) // TOP_K
    slot_tok = jnp.full((n_blocks * MOE_BLOCK,), T, jnp.int32).at[dest].set(tok)
    h_pad = jnp.concatenate([h, jnp.zeros((1, D), h.dtype)], axis=0)
    xs = h_pad[slot_tok].reshape(n_blocks, MOE_BLOCK, D)
    block_e = jnp.minimum(jnp.searchsorted(pad_end, jnp.arange(n_blocks) * MOE_BLOCK, side='right'),
                          N_EXPERTS - 1)

    def expert_block(args):
        xb, e = args
        return (jax.nn.silu(xb @ w1[e]) * (xb @ w3[e])) @ w2[e]

    ys = lax.map(expert_block, (xs, block_e)).reshape(-1, D)
    y = ys[dest].reshape(T, TOP_K, D)
    return jnp.einsum('tk,tkd->td', w.astype(h.dtype), y)


def setup_inputs(seed: int = 0) -> dict:
    key = jax.random.key(seed)
    ks = jax.random.split(key, 32)
    f32 = jnp.float32
    beta_dn = (8.0 * DEPTH) ** -0.25

    def nrm(k, shape, scale):
        return jax.random.normal(k, shape, f32) * scale

    def gain(k, shape):
        return 1.0 + 0.02 * jax.random.normal(k, shape, f32)

    dt = jnp.exp(jax.random.uniform(ks[12], (DEPTH, 2, G_HEADS), f32, math.log(1e-3), math.log(1e-1)))
    return {
        'x': nrm(ks[0], (BATCH, SEQ, D_MODEL), 1.0),
        'c': nrm(ks[1], (BATCH, D_MODEL), 1.0),
        'ctx': nrm(ks[2], (BATCH, CTX_LEN, D_MODEL), 1.0),
        'c_ctx': nrm(ks[3], (D_MODEL,), 1.0),
        'mod_w': nrm(ks[4], (DEPTH, D_MODEL, 6 * D_MODEL), 0.5 * D_MODEL ** -0.5),
        'mod_b': nrm(ks[5], (DEPTH, 6 * D_MODEL), 0.02),
        'w_in': nrm(ks[6], (DEPTH, D_MODEL, N_IN), D_MODEL ** -0.5),
        'conv_w': nrm(ks[7], (DEPTH, CONV_K, 2 * G_QK_WIDTH + G_V_WIDTH), CONV_K ** -0.5),
        'lam_q1': nrm(ks[8], (DEPTH, A_HEAD_DIM), 0.1),
        'lam_k1': nrm(ks[9], (DEPTH, A_HEAD_DIM), 0.1),
        'lam_q2': nrm(ks[10], (DEPTH, A_HEAD_DIM), 0.1),
        'lam_k2': nrm(ks[11], (DEPTH, A_HEAD_DIM), 0.1),
        'subln_g': gain(ks[13], (DEPTH, 2 * A_HEAD_DIM)),
        'a_log': jnp.log(jax.random.uniform(ks[14], (DEPTH, 2, G_HEADS), f32, 1.0, 16.0)),
        'dt_bias': dt + jnp.log(-jnp.expm1(-dt)),
        'onorm_g': gain(ks[15], (DEPTH, G_VAL_DIM)),
        'w_pa': nrm(ks[16], (DEPTH, A_WIDTH, D_MODEL), beta_dn * A_WIDTH ** -0.5),
        'w_pb': nrm(ks[17], (DEPTH, G_V_WIDTH, D_MODEL), beta_dn * G_V_WIDTH ** -0.5),
        'w_o': nrm(ks[18], (DEPTH, D_MODEL, D_MODEL), beta_dn * D_MODEL ** -0.5),
        'ln1_g': gain(ks[19], (DEPTH, D_MODEL)),
        'ln1_b': nrm(ks[20], (DEPTH, D_MODEL), 0.02),
        'router_w': nrm(ks[21], (D_MODEL, N_EXPERTS), D_MODEL ** -0.5),
        'router_b': nrm(ks[22], (N_EXPERTS,), 0.01),
        'w_exp1': nrm(ks[23], (DEPTH, N_EXPERTS, D_MODEL, D_EXPERT), D_MODEL ** -0.5),
        'w_exp3': nrm(ks[24], (DEPTH, N_EXPERTS, D_MODEL, D_EXPERT), D_MODEL ** -0.5),
        'w_exp2': nrm(ks[25], (DEPTH, N_EXPERTS, D_EXPERT, D_MODEL), beta_dn * D_EXPERT ** -0.5),
        'ln2_g': gain(ks[26], (DEPTH, D_MODEL)),
        'ln2_b': nrm(ks[27], (DEPTH, D_MODEL), 0.02),
    }


def reference(x, c, ctx, c_ctx, mod_w, mod_b, w_in, conv_w, lam_q1, lam_k1, lam_q2, lam_k2, subln_g,
              a_log, dt_bias, onorm_g, w_pa, w_pb, w_o, ln1_g, ln1_b, router_w, router_b,
              w_exp1, w_exp3, w_exp2, ln2_g, ln2_b):
    B, S, D = x.shape
    n_ctx = ctx.shape[1]
    alpha = (2.0 * DEPTH) ** 0.25
    cos, sin = axial_rope(S, A_HEAD_DIM)
    cos, sin = cos.astype(x.dtype), sin.astype(x.dtype)
    xc = ctx
    for l in range(DEPTH):
        last = l == DEPTH - 1
        lam_init = 0.8 - 0.6 * math.exp(-0.3 * l)
        m_lat = jnp.split(jax.nn.silu(c) @ mod_w[l] + mod_b[l], 6, axis=-1)
        m_ctx = jnp.split(jax.nn.silu(c_ctx) @ mod_w[l] + mod_b[l], 6, axis=-1)

        h_lat = modulate(x, m_lat[0][:, None], m_lat[1][:, None])
        h_ctx = modulate(xc, m_ctx[0], m_ctx[1])
        y_lat, y_ctx = token_mixer(h_lat, h_ctx, cos, sin, lam_init, not last, w_in[l], conv_w[l],
                                   lam_q1[l], lam_k1[l], lam_q2[l], lam_k2[l], subln_g[l], a_log[l],
                                   dt_bias[l], onorm_g[l], w_pa[l], w_pb[l], w_o[l])
        x = layer_norm(alpha * x + m_lat[2][:, None] * y_lat, ln1_g[l], ln1_b[l])

        h_lat = modulate(x, m_lat[3][:, None], m_lat[4][:, None])
        if last:
            y_lat = moe_ffn(h_lat.reshape(-1, D), router_w, router_b,
                            w_exp1[l], w_exp3[l], w_exp2[l]).reshape(B, S, D)
        else:
            xc = layer_norm(alpha * xc + m_ctx[2] * y_ctx, ln1_g[l], ln1_b[l])
            h_ctx = modulate(xc, m_ctx[3], m_ctx[4])
            tokens = jnp.concatenate([h_ctx.reshape(-1, D), h_lat.reshape(-1, D)], axis=0)
            y = moe_ffn(tokens, router_w, router_b, w_exp1[l], w_exp3[l], w_exp2[l])
            y_ctx = y[:B * n_ctx].reshape(B, n_ctx, D)
            y_lat = y[B * n_ctx:].reshape(B, S, D)
            xc = layer_norm(alpha * xc + m_ctx[5] * y_ctx, ln2_g[l], ln2_b[l])
        x = layer_norm(alpha * x + m_lat[5][:, None] * y_lat, ln2_g[l], ln2_b[l])
    return x
```

```python
import math
from contextlib import ExitStack
import numpy as np
import concourse.bass as bass
import concourse.mybir as mybir
from concourse.bass_utils import run_bass_kernel_spmd

F32 = mybir.dt.float32
BF16 = mybir.dt.bfloat16
AF = mybir.ActivationFunctionType
ALU = mybir.AluOpType
AX = mybir.AxisListType

DEPTH = 4
D = 1024
S = 4096
NCTX = 256
TOK = S + NCTX
NT = TOK // 128
EPS = 1e-6
ALPHA = (2.0 * DEPTH) ** 0.25
NEGBIG = -30000.0
VP = 129
NCOLS = 512 * 9 + 16 + 2048
C_QA, C_QAP, C_KA, C_KAP, C_VA, C_GQ, C_GK, C_GV, C_Z, C_AB, C_GATE = 0, 512, 1024, 1536, 2048, 2560, 3072, 3584, 4096, 4608, 4624
BLOCKS = [(i * 512, 512, False) for i in range(8)] + [(S, 256, True)]

ENGS = ("pe", "dve", "act", "pool", "sp")
ENGATTR = {"pe": "tensor", "dve": "vector", "act": "scalar", "pool": "gpsimd", "sp": "sync"}


class T:
    def __init__(self, t, k):
        self.t = t
        self.k = k

    def __getitem__(self, idx):
        return self.t[idx]


RECYCLE_SEMS = False


class KB:
    def __init__(self, nc):
        self.nc = nc
        self.es = ExitStack()
        self.sem = {}
        self.cnt = {}
        for e in ENGS:
            self.sem[e] = self.es.enter_context(nc.semaphore("s_" + e))
            self.cnt[e] = 0
        self.sem["arrive"] = self.es.enter_context(nc.semaphore("s_arrive"))
        self.sem["go"] = self.es.enter_context(nc.semaphore("s_go"))
        self.epoch = 0
        self.cpool = []
        self.chan = {}
        self.seen = {e: {} for e in ENGS}
        self.last_w = {}
        self.readers = {}
        self.n_ops = 0
        self.uid = 0

    def _semh(self, key):
        return self.sem[key] if key in self.sem else self.cpool[key][0]

    def _deps(self, eng, reads, writes):
        need = {}

        def add(sv):
            if sv is not None and need.get(sv[0], 0) < sv[1]:
                need[sv[0]] = sv[1]
        for r in reads:
            add(self.last_w.get(r))
        for w in writes:
            add(self.last_w.get(w))
            for rd in self.readers.get(w, ()):
                add(rd)
        waits = []
        seen = self.seen[eng]
        for k, v in need.items():
            if seen.get(k, 0) < v:
                seen[k] = v
                waits.append((k, v))
        return waits

    def _commit(self, reads, writes, me):
        for r in reads:
            self.readers.setdefault(r, []).append(me)
        for w in writes:
            self.last_w[w] = me
            self.readers[w] = []

    def _emit(self, eng, fn, waits, inc):
        e = getattr(self.nc, ENGATTR[eng])
        for (k, v) in waits:
            e.wait_ge(self._semh(k), v)
        if fn is not None:
            fn(e).then_inc(self._semh(inc[0]), inc[1])
        self.n_ops += 1

    def op(self, eng, fn, reads=(), writes=()):
        waits = self._deps(eng, reads, writes)
        self.cnt[eng] += 1
        self._emit(eng, fn, waits, (eng, 1))
        self._commit(reads, writes, (eng, self.cnt[eng]))

    def dma(self, q, chan, out, in_, reads=(), writes=(), **kw):
        if chan not in self.chan:
            used = set(self.chan.values())
            idx = None
            for i, ent in enumerate(self.cpool):
                if ent[2] == q and i not in used:
                    idx = i
                    break
            if idx is None:
                idx = len(self.cpool)
                self.cpool.append([self.es.enter_context(self.nc.semaphore("c_%d" % idx)), 0, q])
            self.chan[chan] = idx
        idx = self.chan[chan]
        ch = self.cpool[idx]
        waits = self._deps(q, reads, writes)
        if ch[1] > 0 and self.seen[q].get(idx, 0) < ch[1]:
            self.seen[q][idx] = ch[1]
            waits.append((idx, ch[1]))
        ch[1] += 16
        self._emit(q, lambda e: e.dma_start(out=out, in_=in_, **kw), waits, (idx, 16))
        self._commit(reads, writes, (idx, ch[1]))

    def barrier(self):
        nc = self.nc
        self.epoch += 1
        tot = {e: self.cnt[e] for e in ENGS if self.cnt[e] > 0}
        for idx, ent in enumerate(self.cpool):
            if ent[1] > 0:
                tot[idx] = ent[1]
        for e in ENGS:
            eng = getattr(nc, ENGATTR[e])
            for k, v in tot.items():
                if self.seen[e].get(k, 0) < v:
                    eng.wait_ge(self._semh(k), v)
            if e != "sp":
                eng.sem_inc(self.sem["arrive"], 1)
        sp = nc.sync
        sp.wait_ge(self.sem["arrive"], 4 * self.epoch)
        for k in tot:
            if k in ENGS:
                sp.sem_clear(self._semh(k))
        sp.drain().then_inc(self.sem["go"], 1)
        for e in ENGS:
            if e != "sp":
                getattr(nc, ENGATTR[e]).wait_ge(self.sem["go"], self.epoch)
        for e in ENGS:
            self.cnt[e] = 0
            self.seen[e] = {k: v for k, v in tot.items() if k not in ENGS}
        if RECYCLE_SEMS:
            self.chan = {}
        self.last_w.clear()
        self.readers.clear()

    def finish(self):
        self.barrier()
        self.es.close()


class Pool:
    def __init__(self, kb, tag):
        self.kb = kb
        self.tag = tag
        self.es = ExitStack()

    def sb(self, name, shape, dt=F32):
        self.kb.uid += 1
        t = self.es.enter_context(self.kb.nc.sbuf_tensor("%s_%s_%d" % (self.tag, name, self.kb.uid), list(shape), dt))
        return T(t, name)

    def ps(self, name, shape, dt=F32):
        self.kb.uid += 1
        t = self.es.enter_context(self.kb.nc.psum_tensor("%s_%s_%d" % (self.tag, name, self.kb.uid), list(shape), dt))
        return T(t, name)

    def close(self):
        self.kb.barrier()
        self.es.close()


def mm(kb, out, lhsT, rhs, start, stop, reads, writes, sgc=False):
    if sgc:
        kb.op("pe", lambda e: e.matmul(out, lhsT=lhsT, rhs=rhs, start=start, stop=stop, skip_group_check=True), reads=reads, writes=writes)
    else:
        kb.op("pe", lambda e: e.matmul(out, lhsT=lhsT, rhs=rhs, start=start, stop=stop), reads=reads, writes=writes)


def tr(kb, out, in_, ident, reads, writes):
    kb.op("pe", lambda e: e.transpose(out=out, in_=in_, identity=ident), reads=reads, writes=writes)


def act(kb, out, in_, func, reads, writes, **kw):
    kb.op("act", lambda e: e.activation(out=out, in_=in_, func=func, **kw), reads=reads, writes=writes)


def cp(kb, eng, out, in_, reads, writes):
    if eng == "act":
        act(kb, out, in_, AF.Copy, reads, writes)
    else:
        kb.op(eng, lambda e: e.tensor_copy(out=out, in_=in_), reads=reads, writes=writes)


def tt(kb, eng, out, in0, in1, op, reads, writes):
    kb.op(eng, lambda e: e.tensor_tensor(out=out, in0=in0, in1=in1, op=op), reads=reads, writes=writes)


def ts(kb, eng, out, in0, s1, op0, reads, writes, s2=None, op1=None):
    if op1 is None:
        kb.op(eng, lambda e: e.tensor_scalar(out=out, in0=in0, scalar1=s1, scalar2=None, op0=op0), reads=reads, writes=writes)
    else:
        kb.op(eng, lambda e: e.tensor_scalar(out=out, in0=in0, scalar1=s1, scalar2=s2, op0=op0, op1=op1), reads=reads, writes=writes)


def stt(kb, eng, out, in0, scalar, in1, op0, op1, reads, writes):
    kb.op(eng, lambda e: e.scalar_tensor_tensor(out=out, in0=in0, scalar=scalar, in1=in1, op0=op0, op1=op1), reads=reads, writes=writes)


class Ctx:
    pass


GDBG = {}


def load_w_bf16(kb, C, P, dst, dcol0, src, ncols, nk, stage_ring, cnt, pw=256):
    c = 0
    while c < ncols:
        w = min(pw, ncols - c)
        st = stage_ring[cnt[0] % len(stage_ring)]
        kb.dma("sp", st.k, st[:, 0:nk, 0:w], src[:, c:c + w].rearrange("(k p) n -> p k n", p=128), writes=[st.k])
        eng = ("pool", "dve")[cnt[0] % 2]
        cp(kb, eng, dst[:, 0:nk, dcol0 + c:dcol0 + c + w], st[:, 0:nk, 0:w], [st.k], [dst.k])
        cnt[0] += 1
        c += w


STAGE_IN = {
    "A": ["x_in", "cvec", "mod_w", "mod_bT", "w_in", "cw", "alog", "dtb", "cmat", "ropet"],
    "G0": ["gb_s", "kv_s", "qkgT_s", "onormg", "cmat"],
    "G1": ["gb_s", "kv_s", "qkgT_s", "oA_s", "zs_s", "onormg", "cmat"],
    "T": ["qT_s", "kT_s", "V_s", "lamv", "sublng", "lam_in", "cmat"],
    "M": ["modT_i", "x_in", "hT_s", "yaT_s", "ybT_s", "w_pa", "w_pb", "w_gate", "w_o", "lnp", "cmat"],
    "E": ["modT_i", "x1_s", "router_w", "router_b", "w_e1", "w_e3", "w_e2", "lnp", "cmat", "sel16"],
}
STAGE_OUT = {
    "A": ["modT_o", "hT_s", "qT_s", "kT_s", "V_s", "zs_s", "gb_s", "qkgT_s", "kv_s"],
    "G0": ["oA_s"],
    "G1": ["ybT_s"],
    "T": ["yaT_s"],
    "M": ["x1_s"],
    "E": ["xsA"],
}


def build_program(stage, dbg=(), stop_after=None):
    nc = bass.Bass("TRN2", target_bir_lowering=False)
    kb = KB(nc)
    C = Ctx()
    C.nc, C.kb = nc, kb
    NL = 1

    def din(name, shape, dt=F32):
        if name in STAGE_IN[stage]:
            kind = "ExternalInput"
        elif name in STAGE_OUT[stage] or name in dbg:
            kind = "ExternalOutput"
        else:
            kind = "Internal"
        return nc.dram_tensor(name, list(shape), dt, kind=kind).ap()

    dscr = din

    x_in = din("x_in", [TOK, D])
    cvec = din("cvec", [128, 8, 2])
    mod_w = din("mod_w", [NL, D, 6 * D])
    mod_bT = din("mod_bT", [NL, 128, 48])
    w_in = din("w_in", [NL, D, C_GATE])
    w_gate = din("w_gate", [D, 2048])
    cw_in = din("cw", [NL, 128, 12, 5])
    lamv = din("lamv", [NL, 4, 64])
    lam_in = din("lam_in", [128, 2])
    sublng = din("sublng", [NL, 128])
    onormg = din("onormg", [NL, 128])
    alog = din("alog", [NL, 8])
    dtb = din("dtb", [NL, 8])
    w_pa = din("w_pa", [NL, 512, D])
    w_pb = din("w_pb", [NL, 512, D])
    w_o = din("w_o", [NL, D, D])
    lnp = din("lnp", [NL, 4, D])
    router_w = din("router_w", [D, 16])
    router_b = din("router_b", [1, 16])
    w_e1 = din("w_e1", [NL, 16, D, 512])
    w_e3 = din("w_e3", [NL, 16, D, 512])
    w_e2 = din("w_e2", [NL, 16, 512, D])
    cmat_in = din("cmat", [128, 10, 128])
    ropet = din("ropet", [2, 128, S])
    sel16_in = din("sel16", [16, 16, 128])
    modT_i = din("modT_i", [128, 48, 2])
    modT_o = din("modT_o", [128, 48, 2])

    xsA = dscr("xsA", [TOK, D])
    xsB = dscr("xsB", [TOK, D])
    x1_s = dscr("x1_s", [TOK, D])
    hT_s = dscr("hT_s", [128, 8, TOK], BF16)
    qT_s = dscr("qT_s", [128, 4, TOK], BF16)
    kT_s = dscr("kT_s", [128, 4, TOK], BF16)
    V_s = dscr("V_s", [TOK, 4, VP], BF16)
    zs_s = dscr("zs_s", [TOK, 512])
    gb_s = dscr("gb_s", [TOK, 16])
    qkgT_s = dscr("qkgT_s", [128, 8, TOK])
    kv_s = dscr("kv_s", [TOK, 8, 128])
    oA_s = dscr("oA_s", [TOK, 512])
    ybT_s = dscr("ybT_s", [128, 4, TOK], BF16)
    yaT_s = dscr("yaT_s", [128, 4, TOK], BF16)
    wtT_s = dscr("wtT_s", [16, TOK])
    dbg_mod = dbg_ymix = dbg_ymoe = dbg_oB = dbg_yb = dbg_ya = None
    out = None

    G = Pool(kb, "G")
    cmat = G.sb("cmat", [128, 10, 128])
    kb.dma("sp", "cmat", cmat[:], cmat_in, writes=[cmat.k])
    ident = cmat[:, 0, :]
    ones = cmat[:, 1, :]
    INCL = [cmat[:, 2, :], cmat[:, 3, :]]
    NEGM = [cmat[:, 4, :], cmat[:, 5, :]]
    STRICT = [cmat[:, 6, :], cmat[:, 7, :]]
    CHSEL = cmat[:, 8, 0:2]
    BLK = cmat[:, 9, :]
    CK = cmat.k
    modT = [G.sb("modT%d" % l, [128, 48, 2]) for l in range(NL)]
    epsT = G.sb("epsT", [128, 1])
    kb.op("dve", lambda e: e.memset(epsT[:], EPS), writes=[epsT.k])
    sel16 = G.sb("sel16", [16, 16, 128])
    rw = G.sb("rw", [128, 8, 16])
    rb_bc = G.sb("rb_bc", [128, 16])
    if stage == "E":
        kb.dma("sp", "sel16", sel16[:], sel16_in, writes=[sel16.k])
        kb.dma("sp", "rw", rw[:], router_w.rearrange("(k p) e -> p k e", p=128), writes=[rw.k], allow_slow_non_contiguous=True)
        kb.dma("sp", "rb_bc", rb_bc[:], router_b.partition_broadcast(128), writes=[rb_bc.k])

    P = Pool(kb, "pro")
    NLp = NL if stage == "A" else 0
    sT = P.sb("sT", [128, 8, 2])
    cv = P.sb("cv", [128, 8, 2])
    mbT = P.sb("mbT", [128, NL, 48])
    if stage == "A":
        kb.dma("sp", "cv", cv[:], cvec, writes=[cv.k])
        act(kb, sT[:], cv[:], AF.Silu, [cv.k], [sT.k])
        for l in range(NL):
            kb.dma("sp", "mbT", mbT[:, l, :], mod_bT[l], writes=[mbT.k])
    wst = [P.sb("wst%d" % i, [128, 8, 512]) for i in range(3)]
    pp = [P.ps("pp%d" % i, [128, 8]) for i in range(2)]
    it = 0
    for l in range(NLp):
        for cb in range(12):
            st = wst[it % 3]
            ps = pp[it % 2]
            kb.dma("sp", st.k, st[:], mod_w[l][:, cb * 512:(cb + 1) * 512].rearrange("(k p) n -> p k n", p=128), writes=[st.k])
            for fc in range(4):
                for k in range(8):
                    mm(kb, ps[:, fc * 2:fc * 2 + 2], st[:, k, fc * 128:(fc + 1) * 128], sT[:, k, :], k == 0, k == 7,
                       [st.k, sT.k], [ps.k])
            tt(kb, "dve", modT[l][:, cb * 4:cb * 4 + 4, :], ps[:].rearrange("p (c j) -> p c j", j=2),
               mbT[:, l, cb * 4:cb * 4 + 4].unsqueeze(2).to_broadcast([128, 4, 2]), ALU.add, [ps.k, mbT.k], [modT[l].k])
            it += 1
        if dbg_mod is not None:
            kb.dma("sp", "dbgmod", dbg_mod[l], modT[l][:], reads=[modT[l].k], writes=["dbgmod"])
    if stage == "A":
        kb.dma("sp", "modTo", modT_o, modT[0][:], reads=[modT[0].k], writes=["modTo"])
    elif stage in ("M", "E"):
        kb.dma("sp", "modTi", modT[0][:], modT_i, writes=[modT[0].k])
    P.close()

    xs_cur = x_in
    xs_bufs = [xsA, xsB]
    for l in range(NL):
        xs_next = xsA
        last = False

        LP = Pool(kb, "lc%d" % l)
        sc1 = LP.sb("sc1", [128, 8, 2])
        sc4 = LP.sb("sc4", [128, 8, 2])
        if stage in ("A", "M", "E"):
            ts(kb, "dve", sc1[:], modT[l][:, 8:16, :], 1.0, ALU.add, [modT[l].k], [sc1.k])
            ts(kb, "dve", sc4[:], modT[l][:, 32:40, :], 1.0, ALU.add, [modT[l].k], [sc4.k])
        sh0 = modT[l][:, 0:8, :]
        sh3 = modT[l][:, 24:32, :]
        gbc = {}
        for nm in ({"M": ("m2",), "E": ("m5",)}.get(stage, ())):
            for j in range(2):
                gbc[(nm, j)] = LP.sb("%s_%d" % (nm, j), [128, D])
        lnbc = LP.sb("lnbc", [128, 4, D])
        Pg = Pool(kb, "gbc%d" % l)
        _do_gbc = stage in ("M", "E")
        dg = [Pg.sb("dg%d" % i, [128, 128]) for i in range(2)]
        pg = [Pg.ps("pg%d" % i, [128, 512]) for i in range(2)]
        n = 0
        for (nm, c0) in ({"M": (("m2", 16),), "E": (("m5", 40),)}.get(stage, ())):
            for j in range(2):
                t = gbc[(nm, j)]
                for half in range(2):
                    ps = pg[n % 2]
                    for kk in range(4):
                        k = half * 4 + kk
                        d_ = dg[(n * 4 + kk) % 2]
                        ts(kb, "dve", d_[:], ident, modT[l][:, c0 + k, j:j + 1], ALU.mult, [CK, modT[l].k], [d_.k])
                        mm(kb, ps[:, kk * 128:(kk + 1) * 128], ones, d_[:], True, True, [CK, d_.k], [ps.k])
                    cp(kb, "act", t[:, half * 512:(half + 1) * 512], ps[:], [ps.k], [t.k])
                    n += 1
        Pg.close()
        for i in (range(4) if _do_gbc else ()):
            kb.dma("sp", "lnbc%d" % i, lnbc[:, i, :], lnp[l][i:i + 1, :].partition_broadcast(128), writes=[lnbc.k + str(i)])
        LNK = [lnbc.k + str(i) for i in range(4)]

        def make_hT(tag, x_src, scale, shift, hT_dst, want_router=False):
            Pa = Pool(kb, tag)
            xt = [Pa.sb("xt%d" % i, [128, D]) for i in range(8)]
            ptr = [Pa.ps("ptr%d" % i, [128, 512]) for i in range(4)]
            hb = [Pa.sb("hb%d" % i, [128, 8, 512], BF16) for i in range(2)]
            h32 = [Pa.sb("h32_%d" % i, [128, 8, 512]) for i in range(2)] if want_router else None
            prl = [Pa.ps("prl%d" % i, [128, 16]) for i in range(2)] if want_router else None
            ptw = Pa.ps("ptw", [16, 128]) if want_router else None
            rt = {}
            if want_router:
                for nm, shp in (("sc", [128, 16]), ("sel", [128, 16]), ("m1", [128, 4]), ("ge", [128, 16]), ("s2", [128, 16]),
                                ("m2", [128, 4]), ("gs", [128, 4]), ("gm", [128, 1]), ("gmask", [128, 4]), ("selm", [128, 16]),
                                ("t1", [128, 1]), ("k1", [128, 16]), ("selm2", [128, 16]), ("t2", [128, 1]), ("k2", [128, 16]),
                                ("ch", [128, 16]), ("w", [128, 16]), ("ws", [128, 1]), ("wr", [128, 1]), ("wt", [128, 16]),
                                ("wtT", [16, 128])):
                    rt[nm] = [Pa.sb("r_%s%d" % (nm, i), shp) for i in range(2)]
            xi = 0
            for bi, (t0, bs, isc) in enumerate(BLOCKS):
                j = 1 if isc else 0
                ntl = bs // 128
                tiles = []
                for q in range(ntl):
                    x_ = xt[xi % 8]
                    xi += 1
                    kb.dma("sp", x_.k, x_[:], x_src[t0 + q * 128:t0 + (q + 1) * 128, :], writes=[x_.k])
                    tiles.append(x_)
                h_ = hb[bi % 2]
                for k in range(8):
                    ps = ptr[k % 4]
                    for q in range(ntl):
                        tr(kb, ps[:, q * 128:(q + 1) * 128], tiles[q][:, k * 128:(k + 1) * 128], ident, [tiles[q].k, CK], [ps.k])
                    act(kb, h_[:, k, 0:bs], ps[:, 0:bs], AF.Identity, [ps.k, scale.k, shift.k], [h_.k],
                        scale=scale[:, k, j:j + 1], bias=shift[:, k, j:j + 1])
                    if want_router:
                        h3 = h32[bi % 2]
                        act(kb, h3[:, k, 0:bs], ps[:, 0:bs], AF.Identity, [ps.k, scale.k, shift.k], [h3.k],
                            scale=scale[:, k, j:j + 1], bias=shift[:, k, j:j + 1])
                kb.dma("pool", "st_" + h_.k, hT_dst[:, :, t0:t0 + bs], h_[:, :, 0:bs], reads=[h_.k], writes=["hTdst"])
                if want_router:
                    h3 = h32[bi % 2]
                    for q in range(ntl):
                        ti = (t0 // 128) + q
                        r = {nm: v[ti % 2] for nm, v in rt.items()}
                        pl = prl[ti % 2]
                        for k in range(8):
                            mm(kb, pl[:], h3[:, k, q * 128:(q + 1) * 128], rw[:, k, :], k == 0, k == 7, [h3.k, rw.k], [pl.k])
                        act(kb, r["sc"][:], pl[:], AF.Sigmoid, [pl.k], [r["sc"].k])
                        tt(kb, "dve", r["sel"][:], r["sc"][:], rb_bc[:], ALU.add, [r["sc"].k, rb_bc.k], [r["sel"].k])
                        if GDBG.get("rstage", 99) < 1:
                            continue
                        sel3 = r["sel"][:].rearrange("p (g k) -> p g k", g=4)
                        kb.op("dve", lambda e, o=r["m1"][:], i=sel3: e.tensor_reduce(out=o, in_=i, axis=AX.X, op=ALU.max),
                              reads=[r["sel"].k], writes=[r["m1"].k])
                        tt(kb, "dve", r["ge"][:].rearrange("p (g k) -> p g k", g=4), sel3,
                           r["m1"][:].unsqueeze(2).to_broadcast([128, 4, 4]), ALU.is_ge, [r["sel"].k, r["m1"].k], [r["ge"].k])
                        stt(kb, "dve", r["s2"][:], r["ge"][:], NEGBIG, r["sel"][:], ALU.mult, ALU.add, [r["ge"].k, r["sel"].k], [r["s2"].k])
                        kb.op("dve", lambda e, o=r["m2"][:], i=r["s2"][:].rearrange("p (g k) -> p g k", g=4):
                              e.tensor_reduce(out=o, in_=i, axis=AX.X, op=ALU.max), reads=[r["s2"].k], writes=[r["m2"].k])
                        tt(kb, "dve", r["gs"][:], r["m1"][:], r["m2"][:], ALU.add, [r["m1"].k, r["m2"].k], [r["gs"].k])
                        kb.op("dve", lambda e, o=r["gm"][:], i=r["gs"][:]: e.tensor_reduce(out=o, in_=i, axis=AX.X, op=ALU.max),
                              reads=[r["gs"].k], writes=[r["gm"].k])
                        ts(kb, "dve", r["gmask"][:], r["gs"][:], r["gm"][:, 0:1], ALU.is_ge, [r["gs"].k, r["gm"].k], [r["gmask"].k])
                        if GDBG.get("rstage", 99) < 2:
                            continue
                        ts(kb, "dve", r["gs"][:], r["gmask"][:], -1.0, ALU.add, [r["gmask"].k], [r["gs"].k], s2=-NEGBIG, op1=ALU.mult)
                        tt(kb, "dve", r["selm"][:].rearrange("p (g k) -> p g k", g=4), sel3,
                           r["gs"][:].unsqueeze(2).to_broadcast([128, 4, 4]), ALU.add, [r["sel"].k, r["gs"].k], [r["selm"].k])
                        kb.op("dve", lambda e, o=r["t1"][:], i=r["selm"][:]: e.tensor_reduce(out=o, in_=i, axis=AX.X, op=ALU.max),
                              reads=[r["selm"].k], writes=[r["t1"].k])
                        ts(kb, "dve", r["k1"][:], r["selm"][:], r["t1"][:, 0:1], ALU.is_ge, [r["selm"].k, r["t1"].k], [r["k1"].k])
                        stt(kb, "dve", r["selm2"][:], r["k1"][:], NEGBIG, r["selm"][:], ALU.mult, ALU.add, [r["k1"].k, r["selm"].k], [r["selm2"].k])
                        kb.op("dve", lambda e, o=r["t2"][:], i=r["selm2"][:]: e.tensor_reduce(out=o, in_=i, axis=AX.X, op=ALU.max),
                              reads=[r["selm2"].k], writes=[r["t2"].k])
                        ts(kb, "dve", r["k2"][:], r["selm2"][:], r["t2"][:, 0:1], ALU.is_ge, [r["selm2"].k, r["t2"].k], [r["k2"].k])
                        tt(kb, "dve", r["ch"][:], r["k1"][:], r["k2"][:], ALU.add, [r["k1"].k, r["k2"].k], [r["ch"].k])
                        if GDBG.get("rstage", 99) < 3:
                            continue
                        tt(kb, "dve", r["w"][:], r["ch"][:], r["sc"][:], ALU.mult, [r["ch"].k, r["sc"].k], [r["w"].k])
                        kb.op("dve", lambda e, o=r["ws"][:]: e.memset(o, 0.0), writes=[r["ws"].k])
                        act(kb, r["selm2"][:], r["w"][:], AF.Identity, [r["w"].k], [r["selm2"].k, r["ws"].k], accum_out=r["ws"][:])
                        kb.op("dve", lambda e, o=r["wr"][:], i=r["ws"][:]: e.reciprocal(out=o, in_=i), reads=[r["ws"].k], writes=[r["wr"].k])
                        ts(kb, "dve", r["wt"][:], r["w"][:], r["wr"][:, 0:1], ALU.mult, [r["w"].k, r["wr"].k], [r["wt"].k])
                        if GDBG.get("rstage", 99) < 4:
                            continue
                        mm(kb, ptw[:], r["wt"][:], ident, True, True, [r["wt"].k, CK], [ptw.k])
                        cp(kb, "act", r["wtT"][:], ptw[:], [ptw.k], [r["wtT"].k])
                        kb.dma("pool", "st_wtT%d" % (ti % 2), wtT_s[:, ti * 128:(ti + 1) * 128], r["wtT"][:], reads=[r["wtT"].k], writes=["wtTdst"])
            Pa.close()

        if stage == "A":
            make_hT("A1_%d" % l, xs_cur, sc1, modT[l], hT_s)
        if stop_after == "A1":
            break

        for part in (("a", "b") if stage == "A" else ()):
            Pa = Pool(kb, "A2%s_%d" % (part, l))
            wstage = [Pa.sb("wstg%d" % i, [128, 8, 256]) for i in range(2)]
            wcnt = [0]
            hw = [Pa.sb("hw%d" % i, [128, 8, 516], BF16) for i in range(2)]
            pH = Pa.ps("pH", [128, 16])
            if part == "a":
                Wqk = Pa.sb("Wqk", [128, 8, 2048], BF16)
                Wv = Pa.sb("Wv", [128, 8, 512], BF16)
                Wz = Pa.sb("Wz", [128, 8, 512], BF16)
                Wab = Pa.sb("Wab", [128, 8, 16], BF16)
                load_w_bf16(kb, C, Pa, Wqk, 0, w_in[l][:, C_QA:C_QA + 2048], 2048, 8, wstage, wcnt)
                load_w_bf16(kb, C, Pa, Wv, 0, w_in[l][:, C_VA:C_VA + 512], 512, 8, wstage, wcnt)
                load_w_bf16(kb, C, Pa, Wz, 0, w_in[l][:, C_Z:C_Z + 512], 512, 8, wstage, wcnt)
                load_w_bf16(kb, C, Pa, Wab, 0, w_in[l][:, C_AB:C_AB + 16], 16, 8, wstage, wcnt)
                dtb_bc = Pa.sb("dtb_bc", [128, 8])
                kb.dma("sp", "dtb_bc", dtb_bc[:], dtb[l:l + 1, :].partition_broadcast(128), writes=[dtb_bc.k])
                nea = Pa.sb("nea", [128, 8])
                kb.dma("sp", "nea", nea[:], alog[l:l + 1, :].partition_broadcast(128), writes=[nea.k])
                act(kb, nea[:], nea[:], AF.Exp, [nea.k], [nea.k])
                ts(kb, "dve", nea[:], nea[:], -1.0, ALU.mult, [nea.k], [nea.k])
                cosb = [Pa.sb("cos%d" % i, [128, 512]) for i in range(2)]
                sinb = [Pa.sb("sin%d" % i, [128, 512]) for i in range(2)]
                qko = [Pa.sb("qko%d" % i, [128, 4, 512], BF16) for i in range(2)]
                rt1 = [Pa.sb("rt1_%d" % i, [128, 512]) for i in range(2)]
                rt2 = [Pa.sb("rt2_%d" % i, [128, 512]) for i in range(2)]
                vt = [Pa.sb("vt%d" % i, [128, 4, VP], BF16) for i in range(2)]
                for v_ in vt:
                    kb.op("dve", lambda e, v_=v_: e.memset(v_[:], 1.0), writes=[v_.k])
                zt = [Pa.sb("zt%d" % i, [128, 512]) for i in range(2)]
                gbt = [Pa.sb("gbt%d" % i, [128, 16]) for i in range(2)]
                abt = [Pa.sb("abt%d" % i, [128, 8]) for i in range(2)]
                pA = [Pa.ps("pA%d" % i, [128, 512]) for i in range(2)]
                pB = [Pa.ps("pB%d" % i, [128, 512]) for i in range(2)]
                pTok = [Pa.ps("pTok%d" % i, [128, 512]) for i in range(2)]
            else:
                Wg = Pa.sb("Wg", [128, 8, 1536], BF16)
                load_w_bf16(kb, C, Pa, Wg, 0, w_in[l][:, C_GQ:C_GQ + 1536], 1536, 8, wstage, wcnt)
                cwt = Pa.sb("cwt", [128, 12, 5])
                kb.dma("sp", "cwt", cwt[:], cw_in[l], writes=[cwt.k])
                stg = [Pa.sb("stg%d" % i, [128, 516]) for i in range(2)]
                acc = [Pa.sb("acc%d" % i, [128, 512]) for i in range(2)]
                post = [Pa.sb("post%d" % i, [128, 512]) for i in range(2)]
                sq = [Pa.sb("sq%d" % i, [128, 512]) for i in range(2)]
                rs = [Pa.sb("rs%d" % i, [128, 512]) for i in range(2)]
                gT = [Pa.sb("gT%d" % i, [128, 12, 512]) for i in range(2)]
                kvt = [Pa.sb("kvt%d" % i, [128, 8, 128]) for i in range(2)]
                pM = [Pa.ps("pM%d" % i, [128, 512]) for i in range(3)]
                pSS = [Pa.ps("pSS%d" % i, [128, 512]) for i in range(2)]
                pTr = [Pa.ps("pTr%d" % i, [128, 512]) for i in range(2)]
            cnt = {"qk": 0, "tile": 0, "cb": 0, "h": 0, "tr": 0}
            for bi, (t0, bs, isc) in enumerate(BLOCKS):
                ntl = bs // 128
                h_ = hw[bi % 2]
                kb.dma("sp", h_.k, h_[:, :, 2:2 + bs], hT_s[:, :, t0:t0 + bs], reads=["hTdst"], writes=[h_.k])
                if part == "b":
                    if t0 == 0 or t0 == S:
                        kb.op("pool", lambda e, h_=h_: e.memset(h_[:, :, 0:2], 0.0), writes=[h_.k + "L"])
                    else:
                        kb.dma("sp", h_.k + "L", h_[:, :, 0:2], hT_s[:, :, t0 - 2:t0], reads=["hTdst"], writes=[h_.k + "L"])
                    if t0 + bs == S or t0 + bs == TOK:
                        kb.op("pool", lambda e, h_=h_, bs=bs: e.memset(h_[:, :, bs + 2:bs + 4], 0.0), writes=[h_.k + "R"])
                    else:
                        kb.dma("sp", h_.k + "R", h_[:, :, bs + 2:bs + 4], hT_s[:, :, t0 + bs:t0 + bs + 2], reads=["hTdst"], writes=[h_.k + "R"])
                if part == "a":
                    if not isc:
                        cb_, sb_ = cosb[bi % 2], sinb[bi % 2]
                        kb.dma("sp", cb_.k, cb_[:, 0:bs], ropet[0][:, t0:t0 + bs], writes=[cb_.k])
                        kb.dma("sp", sb_.k, sb_[:, 0:bs], ropet[1][:, t0:t0 + bs], writes=[sb_.k])
                    for gi, (c0, dst) in enumerate(((0, qT_s), (1024, kT_s))):
                        o_ = qko[cnt["qk"] % 2]
                        cnt["qk"] += 1
                        for h in range(4):
                            pa_, pb_ = pA[cnt["h"] % 2], pB[cnt["h"] % 2]
                            cnt["h"] += 1
                            for k in range(8):
                                mm(kb, pa_[:, 0:bs], Wqk[:, k, c0 + h * 128:c0 + (h + 1) * 128], h_[:, k, 2:2 + bs], k == 0, k == 7,
                                   [Wqk.k, h_.k], [pa_.k])
                            if isc:
                                cp(kb, "act", o_[:, h, 0:bs], pa_[:, 0:bs], [pa_.k], [o_.k])
                            else:
                                for k in range(8):
                                    mm(kb, pb_[:, 0:bs], Wqk[:, k, c0 + 512 + h * 128:c0 + 512 + (h + 1) * 128], h_[:, k, 2:2 + bs], k == 0, k == 7,
                                       [Wqk.k, h_.k], [pb_.k])
                                a1, a2 = rt1[h % 2], rt2[h % 2]
                                tt(kb, "dve", a1[:, 0:bs], pa_[:, 0:bs], cb_[:, 0:bs], ALU.mult, [pa_.k, cb_.k], [a1.k])
                                tt(kb, "dve", a2[:, 0:bs], pb_[:, 0:bs], sb_[:, 0:bs], ALU.mult, [pb_.k, sb_.k], [a2.k])
                                tt(kb, "pool", o_[:, h, 0:bs], a1[:, 0:bs], a2[:, 0:bs], ALU.add, [a1.k, a2.k], [o_.k])
                        kb.dma("pool", "st_" + o_.k, dst[:, :, t0:t0 + bs], o_[:, :, 0:bs], reads=[o_.k], writes=["qkdst%d" % gi])
                    for q in range(ntl):
                        ti = cnt["tile"]
                        cnt["tile"] += 1
                        lt = slice(2 + q * 128, 2 + (q + 1) * 128)
                        tok = slice(t0 + q * 128, t0 + (q + 1) * 128)
                        v_ = vt[ti % 2]
                        pt0, pt1 = pTok[0], pTok[1]
                        for k in range(8):
                            mm(kb, pt0[:], h_[:, k, lt], Wv[:, k, :], k == 0, k == 7, [h_.k, Wv.k], [pt0.k])
                        cp(kb, "act", v_[:, :, 0:128], pt0[:].rearrange("p (h e) -> p h e", h=4), [pt0.k], [v_.k])
                        kb.dma("pool", "st_" + v_.k, V_s[tok, :, :], v_[:], reads=[v_.k], writes=["Vdst"])
                        z_ = zt[ti % 2]
                        for k in range(8):
                            mm(kb, pt1[:], h_[:, k, lt], Wz[:, k, :], k == 0, k == 7, [h_.k, Wz.k], [pt1.k])
                        act(kb, z_[:], pt1[:], AF.Silu, [pt1.k], [z_.k])
                        kb.dma("pool", "st_" + z_.k, zs_s[tok, :], z_[:], reads=[z_.k], writes=["zsdst"])
                        g_ = gbt[ti % 2]
                        a_ = abt[ti % 2]
                        for k in range(8):
                            mm(kb, pH[:, 0:16], h_[:, k, lt], Wab[:, k, :], k == 0, k == 7, [h_.k, Wab.k], [pH.k])
                        tt(kb, "dve", a_[:], pH[:, 0:8], dtb_bc[:], ALU.add, [pH.k, dtb_bc.k], [a_.k])
                        act(kb, a_[:], a_[:], AF.Exp, [a_.k], [a_.k])
                        act(kb, a_[:], a_[:], AF.Ln, [a_.k], [a_.k], bias=1.0)
                        tt(kb, "dve", g_[:, 0:8], a_[:], nea[:], ALU.mult, [a_.k, nea.k], [g_.k])
                        act(kb, g_[:, 8:16], pH[:, 8:16], AF.Sigmoid, [pH.k], [g_.k])
                        kb.dma("pool", "st_" + g_.k, gb_s[tok, :], g_[:], reads=[g_.k], writes=["gbdst"])
                    continue
                gT_ = gT[bi % 2]
                for cbi in range(12):
                    ci = cnt["cb"]
                    cnt["cb"] += 1
                    pm = pM[ci % 3]
                    for k in range(8):
                        mm(kb, pm[:, 0:bs], Wg[:, k, cbi * 128:(cbi + 1) * 128], h_[:, k, 2:2 + bs], k == 0, k == 7, [Wg.k, h_.k], [pm.k])
                    for k in range(8):
                        mm(kb, pH[:, 0:2], Wg[:, k, cbi * 128:(cbi + 1) * 128], h_[:, k, 0:2], k == 0, k == 7, [Wg.k, h_.k + "L"], [pH.k])
                    for k in range(8):
                        mm(kb, pH[:, 2:4], Wg[:, k, cbi * 128:(cbi + 1) * 128], h_[:, k, bs + 2:bs + 4], k == 0, k == 7, [Wg.k, h_.k + "R"], [pH.k])
                    s_ = stg[ci % 2]
                    cp(kb, "act", s_[:, 2:2 + bs], pm[:, 0:bs], [pm.k], [s_.k])
                    cp(kb, "dve", s_[:, 0:2], pH[:, 0:2], [pH.k], [s_.k])
                    cp(kb, "dve", s_[:, bs + 2:bs + 4], pH[:, 2:4], [pH.k], [s_.k])
                    ac = acc[ci % 2]
                    ts(kb, "pool", ac[:, 0:bs], s_[:, 0:bs], cwt[:, cbi, 0:1], ALU.mult, [s_.k, cwt.k], [ac.k])
                    for kk in range(1, 5):
                        stt(kb, "dve", ac[:, 0:bs], s_[:, kk:kk + bs], cwt[:, cbi, kk:kk + 1], ac[:, 0:bs], ALU.mult, ALU.add,
                            [s_.k, cwt.k, ac.k], [ac.k])
                    if cbi >= 8:
                        act(kb, gT_[:, cbi, 0:bs], ac[:, 0:bs], AF.Silu, [ac.k], [gT_.k])
                    else:
                        po = post[ci % 2]
                        act(kb, po[:, 0:bs], ac[:, 0:bs], AF.Silu, [ac.k], [po.k])
                        sq_ = sq[ci % 2]
                        tt(kb, "dve", sq_[:, 0:bs], po[:, 0:bs], po[:, 0:bs], ALU.mult, [po.k], [sq_.k])
                        pss = pSS[ci % 2]
                        mm(kb, pss[:, 0:bs], ones, sq_[:, 0:bs], True, True, [CK, sq_.k], [pss.k])
                        r_ = rs[ci % 2]
                        act(kb, r_[:, 0:bs], pss[:, 0:bs], AF.Sqrt, [pss.k, epsT.k], [r_.k], bias=epsT[:, 0:1], scale=1.0)
                        kb.op("dve", lambda e, o=r_[:, 0:bs]: e.reciprocal(out=o, in_=o), reads=[r_.k], writes=[r_.k])
                        if cbi < 4:
                            stt(kb, "dve", gT_[:, cbi, 0:bs], po[:, 0:bs], 128.0 ** -0.5, r_[:, 0:bs], ALU.mult, ALU.mult, [po.k, r_.k], [gT_.k])
                        else:
                            tt(kb, "dve", gT_[:, cbi, 0:bs], po[:, 0:bs], r_[:, 0:bs], ALU.mult, [po.k, r_.k], [gT_.k])
                kb.dma("pool", "st_" + gT_.k, qkgT_s[:, :, t0:t0 + bs], gT_[:, 0:8, 0:bs], reads=[gT_.k], writes=["qkgdst"])
                for q in range(ntl):
                    kv_ = kvt[q % 2]
                    for half in range(2):
                        ptr_ = pTr[cnt["tr"] % 2]
                        cnt["tr"] += 1
                        for i in range(4):
                            tr(kb, ptr_[:, i * 128:(i + 1) * 128], gT_[:, 4 + half * 4 + i, q * 128:(q + 1) * 128], ident, [gT_.k, CK], [ptr_.k])
                        cp(kb, ("act", "dve")[half], kv_[:, half * 4:(half + 1) * 4, :], ptr_[:].rearrange("p (h e) -> p h e", h=4), [ptr_.k], [kv_.k])
                    kb.dma("pool", "st_" + kv_.k, kv_s[t0 + q * 128:t0 + (q + 1) * 128, :, :], kv_[:], reads=[kv_.k], writes=["kvdst"])
            Pa.close()
        if stop_after == "A2":
            break

        for dr in ({"G0": (0,), "G1": (1,)}.get(stage, ())):
            Pg = Pool(kb, "G%d_%d" % (dr, l))
            Sst = Pg.sb("Sst", [128, 4, 128])
            kb.op("dve", lambda e: e.memset(Sst[:], 0.0), writes=[Sst.k + "0", Sst.k + "1", Sst.k + "2", Sst.k + "3"])
            on_bc = Pg.sb("on_bc", [128, 128])
            kb.dma("sp", "on_bc", on_bc[:], onormg[l:l + 1, :].partition_broadcast(128), writes=[on_bc.k])
            R2 = lambda nm, shp, dt=F32, n=2: [Pg.sb("%s%d" % (nm, i), shp, dt) for i in range(n)]
            gbl = R2("gbl", [128, 16])
            kvl = R2("kvl", [128, 8, 128])
            qkl = R2("qkl", [128, 8, 128])
            oAl = R2("oAl", [128, 512])
            zsl = R2("zsl", [128, 512])
            Y = R2("Y", [128, 4, 2])
            smS = R2("smS", [128, 16])
            egc = R2("egc", [128, 4])
            ngc = R2("ngc", [128, 4])
            kds = R2("kds", [128, 4])
            GLe = R2("GLe", [128, 8])
            nb4 = R2("nb4", [128, 4])
            ot = R2("ot", [128, 512])
            ybt = R2("ybt", [128, 512])
            ybT = R2("ybT", [128, 4, 128], BF16)
            ssq = R2("ssq", [128, 4])
            junk = R2("junk", [128, 128])
            H = {}
            for nm in ("diag", "pre", "DTi", "Ebc", "gs", "QKm", "Qa", "Qb", "QTa", "QTb", "TTa", "TTb", "kE", "wT", "ub", "qdT", "kdec", "vn"):
                H[nm] = [Pg.sb("%s_h%d" % (nm, h), [128, 128]) for h in range(4)]
            banks = [Pg.ps("bank%d" % i, [128, 512]) for i in range(8)]
            ph = {}
            for bi_, nm in enumerate(("gr", "qk", "rp", "x1", "x2", "ob", "sb")):
                ph[nm] = [T(banks[bi_].t[:, h * 128:(h + 1) * 128], "pbank_" + nm) for h in range(4)]
            psm = T(banks[7].t[:, 0:16], "psm")
            pws = ph["rp"]
            pob = ph["ob"]
            psb = ph["sb"]
            order = ([32, 33] + list(range(32))) if dr == 0 else ([33, 32] + list(range(31, -1, -1)))
            if GDBG.get("ntiles"):
                nt_ = GDBG["ntiles"]
                order = order[:(nt_[dr] if isinstance(nt_, list) else nt_)]
            for it_, ti in enumerate(order):
                tok = slice(ti * 128, (ti + 1) * 128)
                i2 = it_ % 2
                gb_, kv_, qk_ = gbl[i2], kvl[i2], qkl[i2]
                kb.dma("sp", gb_.k, gb_[:], gb_s[tok, :], reads=["gbdst"], writes=[gb_.k])
                kb.dma("sp", kv_.k, kv_[:], kv_s[tok, :, :], reads=["kvdst"], writes=[kv_.k])
                kb.dma("sp", qk_.k, qk_[:], qkgT_s[:, :, tok], reads=["qkgdst"], writes=[qk_.k])
                if dr == 1:
                    oa_, zs_ = oAl[i2], zsl[i2]
                    kb.dma("sp", oa_.k, oa_[:], oA_s[tok, :], reads=["oAdst"], writes=[oa_.k])
                    kb.dma("sp", zs_.k, zs_[:], zs_s[tok, :], reads=["zsdst"], writes=[zs_.k])
                if GDBG.get("zero_gb"):
                    kb.op("dve", lambda e, o=gb_[:]: e.memset(o, 0.0), writes=[gb_.k])
                if GDBG.get("zero_qk"):
                    kb.op("dve", lambda e, o=qk_[:]: e.memset(o, 0.0), writes=[qk_.k])
                g4 = gb_[:, dr * 4:dr * 4 + 4]
                b4 = gb_[:, 8 + dr * 4:8 + dr * 4 + 4]
                if GDBG.get("gstage", 99) < 0:
                    continue
                Y_, sm_, eg_, ng_, kd_, GL_, nb_ = Y[i2], smS[i2], egc[i2], ngc[i2], kds[i2], GLe[i2], nb4[i2]
                for c_ in range(2):
                    ts(kb, "dve", Y_[:, :, c_], g4, CHSEL[:, c_:c_ + 1], ALU.mult, [gb_.k, CK], [Y_.k])
                mm(kb, psm[:, 0:4], INCL[dr], g4, True, True, [CK, gb_.k], [psm.k])
                mm(kb, psm[:, 4:12], ones, Y_[:].rearrange("p h c -> p (h c)"), True, True, [CK, Y_.k], [psm.k])
                mm(kb, psm[:, 12:16], BLK, g4, True, True, [CK, gb_.k], [psm.k])
                cp(kb, "dve", sm_[:], psm[:], [psm.k], [sm_.k])
                if GDBG.get("gstage", 99) < 0.5:
                    continue
                act(kb, eg_[:], sm_[:, 0:4], AF.Exp, [sm_.k], [eg_.k])
                ts(kb, "dve", ng_[:], sm_[:, 0:4], -1.0, ALU.mult, [sm_.k], [ng_.k])
                tt(kb, "dve", kd_[:], sm_[:, 12:16], sm_[:, 0:4], ALU.subtract, [sm_.k], [kd_.k])
                act(kb, kd_[:], kd_[:], AF.Exp, [kd_.k], [kd_.k])
                act(kb, GL_[:], sm_[:, 4:12], AF.Exp, [sm_.k], [GL_.k])
                ts(kb, "dve", nb_[:], b4, -1.0, ALU.mult, [gb_.k], [nb_.k])
                if GDBG.get("gstage", 99) < 1:
                    continue
                HS = range(4)
                kT = lambda h: qk_[:, 4 + h, :]
                qT = lambda h: qk_[:, h, :]
                ktm = lambda h: kv_[:, h, :]
                vtm = lambda h: kv_[:, 4 + h, :]
                sk_ = GDBG.get("g2skip", "")
                for h in HS:
                    if "gram" not in sk_:
                        mm(kb, ph["gr"][h][:], kT(h), kT(h), True, True, [qk_.k], [ph["gr"][h].k])
                    if "qk" not in sk_:
                        mm(kb, ph["qk"][h][:], kT(h), qT(h), True, True, [qk_.k], [ph["qk"][h].k])
                    if "diag" not in sk_:
                        ts(kb, "dve", H["diag"][h][:], ident, sm_[:, h:h + 1], ALU.mult, [CK, sm_.k], [H["diag"][h].k])
                    if "rp" not in sk_:
                        mm(kb, ph["rp"][h][:], ones, H["diag"][h][:], True, True, [CK, H["diag"][h].k], [ph["rp"][h].k])
                if GDBG.get("gstage", 99) < 2:
                    continue
                for h in HS:
                    stt(kb, "dve", H["pre"][h][:], ph["rp"][h][:], ng_[:, h:h + 1], NEGM[dr], ALU.add, ALU.add,
                        [ph["rp"][h].k, ng_.k, CK], [H["pre"][h].k])
                    act(kb, H["DTi"][h][:], H["pre"][h][:], AF.Exp, [H["pre"][h].k], [H["DTi"][h].k])
                    act(kb, H["Ebc"][h][:], ph["rp"][h][:], AF.Exp, [ph["rp"][h].k], [H["Ebc"][h].k])
                    tt(kb, "dve", H["gs"][h][:], ph["gr"][h][:], STRICT[dr], ALU.mult, [ph["gr"][h].k, CK], [H["gs"][h].k])
                if GDBG.get("gstage", 99) < 3:
                    continue
                for h in HS:
                    stt(kb, "dve", H["Qa"][h][:], H["gs"][h][:], nb_[:, h:h + 1], H["DTi"][h][:], ALU.mult, ALU.mult,
                        [H["gs"][h].k, nb_.k, H["DTi"][h].k], [H["Qa"][h].k])
                    tt(kb, "dve", H["QKm"][h][:], ph["qk"][h][:], H["DTi"][h][:], ALU.mult, [ph["qk"][h].k, H["DTi"][h].k], [H["QKm"][h].k])
                    tr(kb, ph["x1"][h][:], H["Qa"][h][:], ident, [H["Qa"][h].k, CK], [ph["x1"][h].k])
                    tt(kb, "dve", H["TTa"][h][:], H["Qa"][h][:], ident, ALU.add, [H["Qa"][h].k, CK], [H["TTa"][h].k])
                    tt(kb, "dve", H["qdT"][h][:], qT(h), H["Ebc"][h][:], ALU.mult, [qk_.k, H["Ebc"][h].k], [H["qdT"][h].k])
                    act(kb, H["kE"][h][:], ktm(h), AF.Copy, [kv_.k, eg_.k], [H["kE"][h].k], scale=eg_[:, h:h + 1])
                    act(kb, H["kdec"][h][:], ktm(h), AF.Copy, [kv_.k, kd_.k], [H["kdec"][h].k], scale=kd_[:, h:h + 1])
                for h in HS:
                    cp(kb, "act", H["QTa"][h][:], ph["x1"][h][:], [ph["x1"][h].k], [H["QTa"][h].k])
                if GDBG.get("gstage", 99) < 4:
                    continue
                Qc, QTc, TTc = "Qa", "QTa", "TTa"
                Qn, QTn, TTn = "Qb", "QTb", "TTb"
                for step in range(1, 6):
                    for h in HS:
                        mm(kb, ph["x1"][h][:], H[Qc][h][:], H[QTc][h][:], True, True, [H[Qc][h].k, H[QTc][h].k], [ph["x1"][h].k])
                        if step < 5:
                            mm(kb, ph["x2"][h][:], H[QTc][h][:], H[Qc][h][:], True, True, [H[Qc][h].k, H[QTc][h].k], [ph["x2"][h].k])
                    for h in HS:
                        cp(kb, "act", H[QTn][h][:], ph["x1"][h][:], [ph["x1"][h].k], [H[QTn][h].k])
                        if step < 5:
                            cp(kb, "dve", H[Qn][h][:], ph["x2"][h][:], [ph["x2"][h].k], [H[Qn][h].k])
                    for h in HS:
                        mm(kb, ph["x1"][h][:], H[QTn][h][:], H[TTc][h][:], True, True, [H[QTn][h].k, H[TTc][h].k], [ph["x1"][h].k])
                    for h in HS:
                        tt(kb, "dve", H[TTn][h][:], ph["x1"][h][:], H[TTc][h][:], ALU.add, [ph["x1"][h].k, H[TTc][h].k], [H[TTn][h].k])
                    Qc, Qn = Qn, Qc
                    QTc, QTn = QTn, QTc
                    TTc, TTn = TTn, TTc
                if GDBG.get("gstage", 99) < 5:
                    continue
                for h in HS:
                    TT_ = H[TTc][h]
                    mm(kb, ph["x1"][h][:], H["kE"][h][:], TT_[:], True, True, [H["kE"][h].k, TT_.k], [ph["x1"][h].k])
                    mm(kb, ph["x2"][h][:], TT_[:], vtm(h), True, True, [TT_.k, kv_.k], [ph["x2"][h].k])
                for h in HS:
                    cp(kb, "act", H["wT"][h][:], ph["x1"][h][:], [ph["x1"][h].k], [H["wT"][h].k])
                    ts(kb, "dve", H["ub"][h][:], ph["x2"][h][:], b4[:, h:h + 1], ALU.mult, [ph["x2"][h].k, gb_.k], [H["ub"][h].k])
                if GDBG.get("gstage", 99) < 6:
                    continue
                o_ = ot[i2]
                for c in ((0, 1) if dr == 0 else (1, 0)):
                    r = slice(c * 64, (c + 1) * 64)
                    for h in HS:
                        sk = Sst.k + str(h)
                        mm(kb, pws[h][r, :], H["wT"][h][:, r], Sst[:, h, :], True, True, [H["wT"][h].k, sk], [pws[h].k])
                    for h in HS:
                        stt(kb, "dve", H["vn"][h][r, :], pws[h][r, :], nb_[r, h:h + 1], H["ub"][h][r, :], ALU.mult, ALU.add,
                            [pws[h].k, nb_.k, H["ub"][h].k], [H["vn"][h].k])
                    for h in HS:
                        sk = Sst.k + str(h)
                        mm(kb, pob[h][r, :], H["qdT"][h][:, r], Sst[:, h, :], True, False, [H["qdT"][h].k, sk], [pob[h].k])
                        mm(kb, pob[h][r, :], H["QKm"][h][r, r], H["vn"][h][r, :], False, True, [H["QKm"][h].k, H["vn"][h].k], [pob[h].k])
                        mm(kb, psb[h][:], H["kdec"][h][r, :], H["vn"][h][r, :], True, True, [H["kdec"][h].k, H["vn"][h].k], [psb[h].k])
                    for h in HS:
                        sk = Sst.k + str(h)
                        stt(kb, "dve", Sst[:, h, :], Sst[:, h, :], GL_[:, h * 2 + c:h * 2 + c + 1], psb[h][:], ALU.mult, ALU.add,
                            [sk, GL_.k, psb[h].k], [sk])
                        if dr == 0:
                            cp(kb, "act", o_[r, h * 128:(h + 1) * 128], pob[h][r, :], [pob[h].k], [o_.k])
                        else:
                            tt(kb, "dve", o_[r, h * 128:(h + 1) * 128], pob[h][r, :], oa_[r, h * 128:(h + 1) * 128], ALU.add,
                               [pob[h].k, oa_.k], [o_.k])
                if dr == 0:
                    kb.dma("pool", "st_" + o_.k, oA_s[tok, :], o_[:], reads=[o_.k], writes=["oAdst"])
                else:
                    if dbg_oB is not None:
                        kb.dma("pool", "st_dbgoB", dbg_oB[tok, :], o_[:], reads=[o_.k], writes=["dbgoB"])
                    ss_ = ssq[i2]
                    yb_ = ybt[i2]
                    kb.op("dve", lambda e, o=ss_[:]: e.memset(o, 0.0), writes=[ss_.k])
                    for h in HS:
                        act(kb, junk[h % 2][:], o_[:, h * 128:(h + 1) * 128], AF.Square, [o_.k], [junk[h % 2].k, ss_.k], accum_out=ss_[:, h:h + 1])
                    act(kb, ss_[:], ss_[:], AF.Sqrt, [ss_.k, epsT.k], [ss_.k], bias=epsT[:, 0:1], scale=1.0 / 128.0)
                    kb.op("dve", lambda e, o=ss_[:]: e.reciprocal(out=o, in_=o), reads=[ss_.k], writes=[ss_.k])
                    for h in HS:
                        stt(kb, "dve", yb_[:, h * 128:(h + 1) * 128], o_[:, h * 128:(h + 1) * 128], ss_[:, h:h + 1], on_bc[:], ALU.mult, ALU.mult,
                            [o_.k, ss_.k, on_bc.k], [yb_.k])
                    tt(kb, "dve", yb_[:], yb_[:], zs_[:], ALU.mult, [yb_.k, zs_.k], [yb_.k])
                    if dbg_yb is not None:
                        kb.dma("pool", "st_dbgyb", dbg_yb[tok, :], yb_[:], reads=[yb_.k], writes=["dbgyb"])
                    yT_ = ybT[i2]
                    for h in HS:
                        tr(kb, ph["gr"][h][:], yb_[:, h * 128:(h + 1) * 128], ident, [yb_.k, CK], [ph["gr"][h].k])
                    for h in HS:
                        cp(kb, ("act", "dve")[h % 2], yT_[:, h, :], ph["gr"][h][:], [ph["gr"][h].k], [yT_.k])
                    kb.dma("pool", "st_" + yT_.k, ybT_s[:, :, tok], yT_[:], reads=[yT_.k], writes=["ybTdst"])
            Pg.close()
        if stop_after == "G":
            break

        if stage == "T":
            Pt = Pool(kb, "T%d" % l)
            TSK = GDBG.get("tskip", "")
            kTa = Pt.sb("kTa", [128, 4, TOK], BF16)
            Va = Pt.sb("Va", [128, NT, 4, VP], BF16)
            if "kta" not in TSK:
                for h in range(4):
                    for c0 in range(0, TOK, 1088):
                        kb.dma("sp", "kTa%d" % h, kTa[:, h, c0:c0 + 1088], kT_s[:, h, c0:c0 + 1088], reads=["qkdst1"], writes=[kTa.k + str(h)])
            if "va" not in TSK:
                for g in range(0, NT, 2):
                    kb.dma("sp", "Va%d" % (g % 4), Va[:, g:g + 2, :, :], V_s[g * 128:(g + 2) * 128, :, :].rearrange("(t p) h e -> p t h e", p=128),
                           reads=["Vdst"], writes=[Va.k + str(g)])
            KTK = [kTa.k + str(h) for h in range(4)]
            lamc = Pt.sb("lamc", [128, 2])
            kb.dma("sp", "lamc", lamc[:], lam_in, writes=[lamc.k])
            lv = Pt.sb("lv", [128, 4, 64])
            lp = Pt.sb("lp", [128, 2, 64])
            ls = Pt.sb("ls", [128, 2])
            nlam = Pt.sb("nlam", [128, 1])
            if "lam" not in TSK:
                for i in range(4):
                    kb.dma("sp", "lv%d" % i, lv[:, i, :], lamv[l][i:i + 1, :].partition_broadcast(128), writes=[lv.k + str(i)])
                tt(kb, "dve", lp[:, 0, :], lv[:, 0, :], lv[:, 1, :], ALU.mult, [lv.k + "0", lv.k + "1"], [lp.k])
                tt(kb, "dve", lp[:, 1, :], lv[:, 2, :], lv[:, 3, :], ALU.mult, [lv.k + "2", lv.k + "3"], [lp.k])
                kb.op("dve", lambda e: e.tensor_reduce(out=ls[:], in_=lp[:], axis=AX.X, op=ALU.add), reads=[lp.k], writes=[ls.k])
                act(kb, ls[:], ls[:], AF.Exp, [ls.k], [ls.k])
                tt(kb, "dve", nlam[:], ls[:, 1:2], ls[:, 0:1], ALU.subtract, [ls.k], [nlam.k])
                ts(kb, "dve", nlam[:], nlam[:], lamc[:, 0:1], ALU.add, [nlam.k, lamc.k], [nlam.k])
            sg = Pt.sb("sg", [128, 128])
            if "sg" not in TSK:
                kb.dma("sp", "sg", sg[:], sublng[l:l + 1, :].partition_broadcast(128), writes=[sg.k])
                ts(kb, "dve", sg[:], sg[:], lamc[:, 1:2], ALU.mult, [sg.k, lamc.k], [sg.k])
            qb = [[Pt.sb("qb%d_%d" % (i, n_), [128, 4, 512], BF16) for n_ in range(2)] for i in range(2)]
            for i in range(2):
                for n_ in range(2):
                    if "qbz" not in TSK:
                        kb.op("dve", lambda e, t_=qb[i][n_]: e.memset(t_[:], 0.0), writes=[qb[i][n_].k])
            pT = [Pt.sb("pT%d" % i, [128, 512], BF16) for i in range(3)]
            yat = [Pt.sb("yat%d" % i, [128, 512]) for i in range(4)]
            yaT = [Pt.sb("yaT%d" % i, [128, 4, 512], BF16) for i in range(2)]
            t0s = [Pt.sb("t0s%d" % i, [128, 128]) for i in range(2)]
            dd = [Pt.sb("dd%d" % i, [128, 128]) for i in range(2)]
            rr = [Pt.sb("rr%d" % i, [128, 4]) for i in range(2)]
            jk = [Pt.sb("jk%d" % i, [128, 128]) for i in range(2)]
            pst = [Pt.ps("pst%d" % i, [128, 512]) for i in range(2)]
            pacc = [[Pt.ps("pacc%d_%d" % (n_, i), [128, 2, 256]) for i in range(2)] for n_ in range(2)]
            ptr_ = Pt.ps("ptrT", [128, 512])
            ci = 0
            for bi, (t0, bs, isc) in enumerate(BLOCKS):
                if GDBG.get("tblocks") and bi not in GDBG["tblocks"]:
                    continue
                if GDBG.get("tstage", 99) < -1:
                    continue
                ntl = bs // 128
                keyt = [32, 33] if isc else list(range(NT))
                qz = qb[bi % 2]
                for n_ in range(2):
                    rws = slice(n_ * 64, (n_ + 1) * 64)
                    kb.dma("sp", qz[n_].k, qz[n_][rws, :, 0:bs], qT_s[rws, :, t0:t0 + bs], reads=["qkdst0"], writes=[qz[n_].k])
                for h in range(4):
                    for n_ in range(2):
                        rows = slice(n_ * 64, (n_ + 1) * 64)
                        for ki, kt in enumerate(keyt):
                            ps = pst[ci % 2]
                            p_ = pT[ci % 3]
                            ci += 1
                            mm(kb, ps[:, 0:bs], kTa[:, h, kt * 128:(kt + 1) * 128], qz[n_][:, h, 0:bs], True, True, [KTK[h], qz[n_].k], [ps.k])
                            if GDBG.get("tstage", 99) < 0:
                                continue
                            act(kb, p_[:, 0:bs], ps[:, 0:bs], AF.Exp, [ps.k], [p_.k], scale=0.125)
                            if GDBG.get("tstage", 99) < 1:
                                continue
                            for q in range(ntl):
                                pa = pacc[n_][q // 2]
                                mm(kb, pa[:, q % 2, 0:129], p_[:, q * 128:(q + 1) * 128], Va[:, kt, h, 0:129], ki == 0 and q % 2 == 0, ki == len(keyt) - 1,
                                   [p_.k, Va.k + str(kt - kt % 2)], [pa.k], sgc=True)
                    if GDBG.get("tstage", 99) < 2:
                        continue
                    for q in range(ntl):
                        o0 = pacc[0][q // 2]
                        o1 = pacc[1][q // 2]
                        k0, k1 = o0.k, o1.k
                        r_ = rr[q % 2]
                        kb.op("dve", lambda e, o=r_[:, 0:1], i=o0[:, q % 2, 128:129]: e.reciprocal(out=o, in_=i), reads=[k0], writes=[r_.k])
                        kb.op("dve", lambda e, o=r_[:, 1:2], i=o1[:, q % 2, 128:129]: e.reciprocal(out=o, in_=i), reads=[k1], writes=[r_.k])
                        tt(kb, "dve", r_[:, 1:2], r_[:, 1:2], nlam[:], ALU.mult, [r_.k, nlam.k], [r_.k])
                        t_ = t0s[q % 2]
                        act(kb, t_[:], o0[:, q % 2, 0:128], AF.Copy, [k0, r_.k], [t_.k], scale=r_[:, 0:1])
                        d_ = dd[q % 2]
                        stt(kb, "dve", d_[:], o1[:, q % 2, 0:128], r_[:, 1:2], t_[:], ALU.mult, ALU.add, [k1, r_.k, t_.k], [d_.k])
                        kb.op("dve", lambda e, o=r_[:, 2:3]: e.memset(o, 0.0), writes=[r_.k])
                        act(kb, jk[q % 2][:], d_[:], AF.Square, [d_.k], [jk[q % 2].k, r_.k], accum_out=r_[:, 2:3])
                        act(kb, r_[:, 2:3], r_[:, 2:3], AF.Sqrt, [r_.k, epsT.k], [r_.k], bias=epsT[:, 0:1], scale=1.0 / 128.0)
                        kb.op("dve", lambda e, o=r_[:, 2:3]: e.reciprocal(out=o, in_=o), reads=[r_.k], writes=[r_.k])
                        ya_ = yat[q]
                        stt(kb, "dve", ya_[:, h * 128:(h + 1) * 128], d_[:], r_[:, 2:3], sg[:], ALU.mult, ALU.mult, [d_.k, r_.k, sg.k], [ya_.k])
                yT_ = yaT[bi % 2]
                if GDBG.get("tstage", 99) < 3:
                    continue
                for q in range(ntl):
                    if dbg_ya is not None:
                        kb.dma("pool", "st_dbgya", dbg_ya[t0 + q * 128:t0 + (q + 1) * 128, :], yat[q][:], reads=[yat[q].k], writes=["dbgya"])
                    for h in range(4):
                        tr(kb, ptr_[:, h * 128:(h + 1) * 128], yat[q][:, h * 128:(h + 1) * 128], ident, [yat[q].k, CK], [ptr_.k])
                    cp(kb, ("act", "dve")[q % 2], yT_[:, :, q * 128:(q + 1) * 128], ptr_[:].rearrange("p (h e) -> p h e", h=4), [ptr_.k], [yT_.k])
                kb.dma("pool", "st_" + yT_.k, yaT_s[:, :, t0:t0 + bs], yT_[:, :, 0:bs], reads=[yT_.k], writes=["yaTdst"])
            Pt.close()
        if stop_after == "T":
            break

        def layer_norm_out(Pm, ln, z_, gi, bi_, x_out, eng_last="pool"):
            st_, mv_, sd_ = ln
            zz = z_[:].rearrange("p (c f) -> p c f", c=2)
            for c_ in range(2):
                kb.op("dve", lambda e, o=st_[:, c_, :], i=zz[:, c_, :]: e.bn_stats(out=o, in_=i), reads=[z_.k], writes=[st_.k])
            kb.op("dve", lambda e: e.bn_aggr(out=mv_[:], in_=st_[:]), reads=[st_.k], writes=[mv_.k])
            act(kb, sd_[:, 0:1], mv_[:, 1:2], AF.Sqrt, [mv_.k, epsT.k], [sd_.k], bias=epsT[:, 0:1], scale=1.0)
            kb.op("dve", lambda e: e.reciprocal(out=sd_[:, 0:1], in_=sd_[:, 0:1]), reads=[sd_.k], writes=[sd_.k])
            stt(kb, "dve", sd_[:, 1:2], mv_[:, 0:1], -1.0, sd_[:, 0:1], ALU.mult, ALU.mult, [mv_.k, sd_.k], [sd_.k])
            act(kb, z_[:], z_[:], AF.Identity, [z_.k, sd_.k], [z_.k], scale=sd_[:, 0:1], bias=sd_[:, 1:2])
            tt(kb, "dve", z_[:], z_[:], lnbc[:, gi, :], ALU.mult, [z_.k, LNK[gi]], [z_.k])
            tt(kb, eng_last, x_out[:], z_[:], lnbc[:, bi_, :], ALU.add, [z_.k, LNK[bi_]], [x_out.k])

        if stage == "M":
            Pm = Pool(kb, "M%d" % l)
            wstage = [Pm.sb("wstg%d" % i, [128, 8, 256]) for i in range(2)]
            wcnt = [0]
            Wpa = Pm.sb("Wpa", [128, 4, D], BF16)
            Wpb = Pm.sb("Wpb", [128, 4, D], BF16)
            Wgt = Pm.sb("Wgt", [128, 8, 2048], BF16)
            Wo = Pm.sb("Wo", [128, 8, D], BF16)
            load_w_bf16(kb, C, Pm, Wpa, 0, w_pa[l], D, 4, wstage, wcnt)
            load_w_bf16(kb, C, Pm, Wpb, 0, w_pb[l], D, 4, wstage, wcnt)
            load_w_bf16(kb, C, Pm, Wgt, 0, w_gate, 2048, 8, wstage, wcnt)
            load_w_bf16(kb, C, Pm, Wo, 0, w_o[l], D, 8, wstage, wcnt)
            hb = [Pm.sb("hb%d" % i, [128, 8, 512], BF16) for i in range(2)]
            yab = [Pm.sb("yab%d" % i, [128, 4, 512], BF16) for i in range(2)]
            ybb = [Pm.sb("ybb%d" % i, [128, 4, 512], BF16) for i in range(2)]
            mT = [Pm.sb("mT%d" % i, [128, 8, 512], BF16) for i in range(2)]
            sga = [Pm.sb("sga%d" % i, [128, 512]) for i in range(2)]
            sgb = [Pm.sb("sgb%d" % i, [128, 512]) for i in range(2)]
            xt = [Pm.sb("xt%d" % i, [128, D]) for i in range(3)]
            zt_ = [Pm.sb("zt%d" % i, [128, D]) for i in range(3)]
            lnst = [(Pm.sb("st%d" % i, [128, 2, 6]), Pm.sb("mv%d" % i, [128, 2]), Pm.sb("sd%d" % i, [128, 2])) for i in range(2)]
            ppa = Pm.ps("ppa", [128, 512])
            ppb = Pm.ps("ppb", [128, 512])
            pga = Pm.ps("pga", [128, 512])
            pgb = Pm.ps("pgb", [128, 512])
            py = [Pm.ps("py%d" % i, [128, 512]) for i in range(4)]
            tc = 0
            for bi, (t0, bs, isc) in enumerate(BLOCKS):
                if GDBG.get("tblocks") and bi not in GDBG["tblocks"]:
                    continue
                ntl = bs // 128
                jj = 1 if isc else 0
                h_, ya_, yb_, m_ = hb[bi % 2], yab[bi % 2], ybb[bi % 2], mT[bi % 2]
                kb.dma("sp", h_.k, h_[:, :, 0:bs], hT_s[:, :, t0:t0 + bs], reads=["hTdst"], writes=[h_.k])
                kb.dma("sp", ya_.k, ya_[:, :, 0:bs], yaT_s[:, :, t0:t0 + bs], reads=["yaTdst"], writes=[ya_.k])
                kb.dma("sp", yb_.k, yb_[:, :, 0:bs], ybT_s[:, :, t0:t0 + bs], reads=["ybTdst"], writes=[yb_.k])
                for fc in range(8):
                    cs = slice(fc * 128, (fc + 1) * 128)
                    for k in range(4):
                        mm(kb, ppa[:, 0:bs], Wpa[:, k, cs], ya_[:, k, 0:bs], k == 0, k == 3, [Wpa.k, ya_.k], [ppa.k])
                    for k in range(4):
                        mm(kb, ppb[:, 0:bs], Wpb[:, k, cs], yb_[:, k, 0:bs], k == 0, k == 3, [Wpb.k, yb_.k], [ppb.k])
                    for k in range(8):
                        mm(kb, pga[:, 0:bs], Wgt[:, k, cs], h_[:, k, 0:bs], k == 0, k == 7, [Wgt.k, h_.k], [pga.k])
                    for k in range(8):
                        mm(kb, pgb[:, 0:bs], Wgt[:, k, 1024 + fc * 128:1024 + (fc + 1) * 128], h_[:, k, 0:bs], k == 0, k == 7, [Wgt.k, h_.k], [pgb.k])
                    sa, sb2 = sga[fc % 2], sgb[fc % 2]
                    act(kb, sa[:, 0:bs], pga[:, 0:bs], AF.Sigmoid, [pga.k], [sa.k])
                    act(kb, sb2[:, 0:bs], pgb[:, 0:bs], AF.Sigmoid, [pgb.k], [sb2.k])
                    tt(kb, "dve", sa[:, 0:bs], sa[:, 0:bs], ppa[:, 0:bs], ALU.mult, [sa.k, ppa.k], [sa.k])
                    tt(kb, "dve", sb2[:, 0:bs], sb2[:, 0:bs], ppb[:, 0:bs], ALU.mult, [sb2.k, ppb.k], [sb2.k])
                    tt(kb, "pool", m_[:, fc, 0:bs], sa[:, 0:bs], sb2[:, 0:bs], ALU.add, [sa.k, sb2.k], [m_.k])
                for q in range(ntl):
                    tok = slice(t0 + q * 128, t0 + (q + 1) * 128)
                    x_ = xt[tc % 3]
                    z_ = zt_[tc % 3]
                    kb.dma("sp", x_.k, x_[:], xs_cur[tok, :], writes=[x_.k])
                    for half in range(2):
                        ps = py[(tc * 2 + half) % 4]
                        for k in range(8):
                            mm(kb, ps[:], m_[:, k, q * 128:(q + 1) * 128], Wo[:, k, half * 512:(half + 1) * 512], k == 0, k == 7, [m_.k, Wo.k], [ps.k])
                        if dbg_ymix is not None:
                            cp(kb, "act", z_[:, half * 512:(half + 1) * 512], ps[:], [ps.k], [z_.k])
                        else:
                            tt(kb, "dve", z_[:, half * 512:(half + 1) * 512], ps[:], gbc[("m2", jj)][:, half * 512:(half + 1) * 512], ALU.mult,
                               [ps.k, gbc[("m2", jj)].k], [z_.k])
                    if dbg_ymix is not None:
                        kb.dma("pool", "st_dbgymix", dbg_ymix[tok, :], z_[:], reads=[z_.k], writes=["dbgymix"])
                        tt(kb, "dve", z_[:], z_[:], gbc[("m2", jj)][:], ALU.mult, [z_.k, gbc[("m2", jj)].k], [z_.k])
                    stt(kb, "dve", z_[:], x_[:], ALPHA, z_[:], ALU.mult, ALU.add, [x_.k, z_.k], [z_.k])
                    layer_norm_out(Pm, lnst[tc % 2], z_, 0, 1, x_)
                    kb.dma("pool", "st_" + x_.k, x1_s[tok, :], x_[:], reads=[x_.k], writes=["x1dst"])
                    tc += 1
            Pm.close()
        if stop_after == "M":
            break

        class _Sh:
            pass
        if stage == "E":
            make_hT("E1_%d" % l, x1_s, sc4, _ShiftView(modT[l], 24), hT_s, want_router=not GDBG.get("no_router"))

            Pe = Pool(kb, "E2_%d" % l)
            wst1 = [Pe.sb("wst1_%d" % i, [128, 8, 256]) for i in range(2)]
            W1 = [Pe.sb("W1_%d" % i, [128, 8, 512], BF16) for i in range(2)]
            W3 = [Pe.sb("W3_%d" % i, [128, 8, 512], BF16) for i in range(2)]
            W2 = [Pe.sb("W2_%d" % i, [128, 4, D], BF16) for i in range(2)]
            passes = [(0, 12), (12, 11), (23, 11)]
            if GDBG.get("e_passes") is not None:
                passes = passes[:GDBG["e_passes"]]
            hp = Pe.sb("hp", [128, 8, 12 * 128], BF16)
            wtp = Pe.sb("wtp", [16, 12 * 128])
            yacc = Pe.sb("yacc", [128, 12, D])
            aT = [Pe.sb("aT%d" % i, [128, 4, 512], BF16) for i in range(2)]
            s1 = [Pe.sb("s1_%d" % i, [128, 512]) for i in range(2)]
            wb = [Pe.sb("wb%d" % i, [128, 512]) for i in range(2)]
            xt = [Pe.sb("xt%d" % i, [128, D]) for i in range(2)]
            lnst = [(Pe.sb("st%d" % i, [128, 2, 6]), Pe.sb("mv%d" % i, [128, 2]), Pe.sb("sd%d" % i, [128, 2])) for i in range(2)]
            pw = [Pe.ps("pw%d" % i, [128, 512]) for i in range(1)]
            p1 = [Pe.ps("p1_%d" % i, [128, 512]) for i in range(2)]
            p3 = [Pe.ps("p3_%d" % i, [128, 512]) for i in range(2)]
            py = [Pe.ps("py%d" % i, [128, 512]) for i in range(3)]
            wc = [0]
            ec = 0
            for (tile0, ntile) in passes:
                ntok = ntile * 128
                tk0 = tile0 * 128
                kb.dma("sp", "hp", hp[:, :, 0:ntok], hT_s[:, :, tk0:tk0 + ntok], reads=["hTdst"], writes=[hp.k])
                kb.dma("sp", "wtp", wtp[:, 0:ntok], wtT_s[:, tk0:tk0 + ntok], reads=["wtTdst"], writes=[wtp.k])
                subs = []
                o_ = 0
                while o_ < ntok:
                    w_ = min(512, ntok - o_)
                    subs.append((o_, w_))
                    o_ += w_
                for e_ in range(GDBG.get("e_nexp", 16)):
                    W1_, W3_, W2_ = W1[ec % 2], W3[ec % 2], W2[ec % 2]
                    ec += 1
                    load_w_bf16(kb, C, Pe, W1_, 0, w_e1[l][e_], 512, 8, wst1, wc)
                    load_w_bf16(kb, C, Pe, W3_, 0, w_e3[l][e_], 512, 8, wst1, wc)
                    load_w_bf16(kb, C, Pe, W2_, 0, w_e2[l][e_], D, 4, wst1, wc)
                    for si, (o_, w_) in enumerate(subs):
                        a_ = aT[si % 2]
                        wb_ = wb[si % 2]
                        mm(kb, pw[0][:, 0:w_], sel16[:, e_, :], wtp[:, o_:o_ + w_], True, True, [sel16.k, wtp.k], [pw[0].k])
                        cp(kb, "act", wb_[:, 0:w_], pw[0][:, 0:w_], [pw[0].k], [wb_.k])
                        for f in range(4):
                            fs = slice(f * 128, (f + 1) * 128)
                            pp1, pp3 = p1[f % 2], p3[f % 2]
                            for k in range(8):
                                mm(kb, pp1[:, 0:w_], W1_[:, k, fs], hp[:, k, o_:o_ + w_], k == 0, k == 7, [W1_.k, hp.k], [pp1.k])
                            for k in range(8):
                                mm(kb, pp3[:, 0:w_], W3_[:, k, fs], hp[:, k, o_:o_ + w_], k == 0, k == 7, [W3_.k, hp.k], [pp3.k])
                            s_ = s1[f % 2]
                            act(kb, s_[:, 0:w_], pp1[:, 0:w_], AF.Silu, [pp1.k], [s_.k])
                            tt(kb, "dve", s_[:, 0:w_], s_[:, 0:w_], pp3[:, 0:w_], ALU.mult, [s_.k, pp3.k], [s_.k])
                            tt(kb, "pool", a_[:, f, 0:w_], s_[:, 0:w_], wb_[:, 0:w_], ALU.mult, [s_.k, wb_.k], [a_.k])
                        for q in range(w_ // 128):
                            tl = (o_ // 128) + q
                            for half in range(2):
                                ps = py[(q * 2 + half) % 3]
                                for f in range(4):
                                    mm(kb, ps[:], a_[:, f, q * 128:(q + 1) * 128], W2_[:, f, half * 512:(half + 1) * 512], f == 0, f == 3,
                                       [a_.k, W2_.k], [ps.k])
                                yk = yacc.k + str(tl)
                                if e_ == 0:
                                    cp(kb, "act", yacc[:, tl, half * 512:(half + 1) * 512], ps[:], [ps.k], [yk])
                                else:
                                    tt(kb, "dve", yacc[:, tl, half * 512:(half + 1) * 512], ps[:], yacc[:, tl, half * 512:(half + 1) * 512], ALU.add,
                                       [ps.k, yk], [yk])
                for tl in range(ntile):
                    ti = tile0 + tl
                    tok = slice(ti * 128, (ti + 1) * 128)
                    jj = 1 if ti >= 32 else 0
                    x_ = xt[tl % 2]
                    yk = yacc.k + str(tl)
                    kb.dma("sp", x_.k, x_[:], x1_s[tok, :], reads=["x1dst"], writes=[x_.k])
                    if dbg_ymoe is not None:
                        kb.dma("pool", "st_dbgymoe", dbg_ymoe[tok, :], yacc[:, tl, :], reads=[yk], writes=["dbgymoe"])
                    tt(kb, "dve", yacc[:, tl, :], yacc[:, tl, :], gbc[("m5", jj)][:], ALU.mult, [yk, gbc[("m5", jj)].k], [yk])
                    stt(kb, "dve", yacc[:, tl, :], x_[:], ALPHA, yacc[:, tl, :], ALU.mult, ALU.add, [x_.k, yk], [yk])
                    zt = T(yacc.t[:, tl, :], yk)
                    layer_norm_out(Pe, lnst[tl % 2], zt, 2, 3, x_)
                    kb.dma("pool", "st_" + x_.k, xs_next[tok, :], x_[:], reads=[x_.k], writes=["xsdst"])
            Pe.close()
        LP.close()
        xs_cur = xs_next
    kb.finish()
    return nc


class _ShiftView:
    def __init__(self, t, c0):
        self.t = t
        self.c0 = c0
        self.k = t.k

    def __getitem__(self, idx):
        p, k, j = idx
        return self.t[p, self.c0 + k, j]


def _consts():
    i = np.arange(128)
    same = (i[:, None] // 64) == (i[None, :] // 64)
    cm = np.zeros((128, 10, 128), np.float32)
    cm[:, 0] = np.eye(128)
    cm[:, 1] = 1.0
    inclA = same & (i[None, :] >= i[:, None])
    inclB = same & (i[None, :] <= i[:, None])
    cm[:, 2] = inclA
    cm[:, 3] = inclB
    cm[:, 4] = np.where(inclA, 0.0, NEGBIG)
    cm[:, 5] = np.where(inclB, 0.0, NEGBIG)
    cm[:, 6] = inclA & ~np.eye(128, dtype=bool)
    cm[:, 7] = inclB & ~np.eye(128, dtype=bool)
    cm[:, 8, 0] = (i < 64)
    cm[:, 8, 1] = (i >= 64)
    cm[:, 9] = same
    t = np.arange(S)
    row = (t // 64).astype(np.float32)
    col = (t % 64).astype(np.float32)
    inv = (10000.0 ** (-np.arange(16, dtype=np.float32) / 16)).astype(np.float32)
    rt = np.zeros((2, 128, S), np.float32)
    for p in range(128):
        d = p % 64
        a, half, f = d // 32, (d % 32) // 16, d % 16
        ang = ((row if a == 0 else col) * inv[f]).astype(np.float32)
        rt[0, p] = np.cos(ang)
        rt[1, p] = np.sin(ang) * (-1.0 if half == 0 else 1.0)
    sel = np.zeros((16, 16, 128), np.float32)
    for e in range(16):
        sel[e, e, :] = 1.0
    return cm, rt, sel


def _layer_inputs(inp, l):
    w_in = inp["w_in"][l]
    d = np.arange(64)
    partner = np.where((d % 32) < 16, d + 16, d - 16)
    perm = (np.arange(512) // 64) * 64 + partner[np.arange(512) % 64]
    cols = np.concatenate([
        np.arange(0, 512), perm, 512 + np.arange(512), 512 + perm, 1024 + np.arange(512),
        1536 + np.arange(1536), 3072 + np.arange(512), 3584 + np.arange(16)])
    lam_init = 0.8 - 0.6 * math.exp(-0.3 * l)
    c = np.ascontiguousarray
    return {
        "mod_w": c(inp["mod_w"][l:l + 1]),
        "mod_bT": c(inp["mod_b"][l:l + 1].reshape(1, 48, 128).transpose(0, 2, 1)),
        "w_in": c(w_in[:, cols])[None],
        "w_gate": c(w_in[:, 3600:3600 + 2048]),
        "cw": c(inp["conv_w"][l:l + 1].reshape(1, 5, 12, 128).transpose(0, 3, 2, 1)),
        "lamv": c(np.stack([inp["lam_q1"][l], inp["lam_k1"][l], inp["lam_q2"][l], inp["lam_k2"][l]], 0))[None],
        "lam_in": c(np.tile(np.array([[-lam_init, 1.0 - lam_init]], np.float32), (128, 1))),
        "sublng": c(inp["subln_g"][l:l + 1]),
        "onormg": c(inp["onorm_g"][l:l + 1]),
        "alog": c(inp["a_log"][l:l + 1].reshape(1, 8)),
        "dtb": c(inp["dt_bias"][l:l + 1].reshape(1, 8)),
        "w_pa": c(inp["w_pa"][l:l + 1]),
        "w_pb": c(inp["w_pb"][l:l + 1]),
        "w_o": c(inp["w_o"][l:l + 1]),
        "lnp": c(np.stack([inp["ln1_g"][l], inp["ln1_b"][l], inp["ln2_g"][l], inp["ln2_b"][l]], 0))[None],
        "router_w": c(inp["router_w"]),
        "router_b": c(inp["router_b"].reshape(1, 16)),
        "w_e1": c(inp["w_exp1"][l:l + 1]),
        "w_e3": c(inp["w_exp3"][l:l + 1]),
        "w_e2": c(inp["w_exp2"][l:l + 1]),
    }


_PROGS = {}


def _run_stage(stage, shared, per_core, dbg=()):
    key = (stage, tuple(dbg))
    nc = build_program(stage, dbg)
    in_maps = []
    for pc in per_core:
        m = {}
        for name in STAGE_IN[stage]:
            m[name] = pc[name] if name in pc else shared[name]
        in_maps.append(m)
    res = run_bass_kernel_spmd(nc, in_maps, core_ids=list(range(len(per_core))))
    return res.results


def run_layers(inp, layers=range(DEPTH), cores=(0, 1, 2, 3), x_override=None, dbg_hook=None):
    inp = {k: np.asarray(v, dtype=np.float32) for k, v in inp.items()}
    cm, rt, sel = _consts()
    state = []
    for b in cores:
        st = {"x_in": np.ascontiguousarray(np.concatenate([inp["x"][b], inp["ctx"][b]], 0)),
              "cvec": np.ascontiguousarray(np.stack([inp["c"][b].reshape(8, 128).T, inp["c_ctx"].reshape(8, 128).T], -1))}
        state.append(st)
    for l in layers:
        sh = _layer_inputs(inp, l)
        sh.update({"cmat": cm, "ropet": rt, "sel16": sel})
        r = _run_stage("A", sh, state)
        for st, o in zip(state, r):
            for k in STAGE_OUT["A"]:
                st[k if k != "modT_o" else "modT_i"] = np.asarray(o[k])
        r = _run_stage("G0", sh, state)
        for st, o in zip(state, r):
            st["oA_s"] = np.asarray(o["oA_s"])
        r = _run_stage("G1", sh, state)
        for st, o in zip(state, r):
            st["ybT_s"] = np.asarray(o["ybT_s"])
        r = _run_stage("T", sh, state)
        for st, o in zip(state, r):
            st["yaT_s"] = np.asarray(o["yaT_s"])
        r = _run_stage("M", sh, state)
        for st, o in zip(state, r):
            st["x1_s"] = np.asarray(o["x1_s"])
        r = _run_stage("E", sh, state)
        for st, o in zip(state, r):
            st["x_in"] = np.asarray(o["xsA"], dtype=np.float32)
        if dbg_hook is not None:
            dbg_hook(l, state)
    return state


def kernel(**inputs):
    state = run_layers(inputs)
    return np.stack([np.ascontiguousarray(st["x_in"][:S], dtype=np.float32) for st in state], 0)
```

```python
import math
from contextlib import ExitStack
import numpy as np
import concourse.bass as bass
import concourse.mybir as mybir
from concourse.bass_utils import run_bass_kernel_spmd

F32 = mybir.dt.float32
BF16 = mybir.dt.bfloat16
AF = mybir.ActivationFunctionType
ALU = mybir.AluOpType
AX = mybir.AxisListType

DEPTH = 4
D = 1024
S = 4096
NCTX = 256
TOK = S + NCTX
NT = TOK // 128
EPS = 1e-6
ALPHA = (2.0 * DEPTH) ** 0.25
NEGBIG = -30000.0
VP = 129
NCOLS = 512 * 9 + 16 + 2048
C_QA, C_QAP, C_KA, C_KAP, C_VA, C_GQ, C_GK, C_GV, C_Z, C_AB, C_GATE = 0, 512, 1024, 1536, 2048, 2560, 3072, 3584, 4096, 4608, 4624
BLOCKS = [(i * 512, 512, False) for i in range(8)] + [(S, 256, True)]
QT = 2048 + NCTX
BLOCKS_T = [(i * 512, 512, False) for i in range(4)] + [(2048, 256, True)]
TE = 2048 + 128
BLOCKS_E = [(i * 512, 512, False) for i in range(4)] + [(2048, 128, True)]

ENGS = ("pe", "dve", "act", "pool", "sp")
ENGATTR = {"pe": "tensor", "dve": "vector", "act": "scalar", "pool": "gpsimd", "sp": "sync"}


class T:
    def __init__(self, t, k):
        self.t = t
        self.k = k

    def __getitem__(self, idx):
        return self.t[idx]


RECYCLE_SEMS = False


class KB:
    def __init__(self, nc):
        self.nc = nc
        self.es = ExitStack()
        self.sem = {}
        self.cnt = {}
        for e in ENGS:
            self.sem[e] = self.es.enter_context(nc.semaphore("s_" + e))
            self.cnt[e] = 0
        self.sem["arrive"] = self.es.enter_context(nc.semaphore("s_arrive"))
        self.sem["go"] = self.es.enter_context(nc.semaphore("s_go"))
        self.epoch = 0
        self.cpool = []
        self.chan = {}
        self.seen = {e: {} for e in ENGS}
        self.last_w = {}
        self.readers = {}
        self.n_ops = 0
        self.uid = 0

    def _semh(self, key):
        return self.sem[key] if key in self.sem else self.cpool[key][0]

    def _deps(self, eng, reads, writes):
        need = {}

        def add(sv):
            if sv is not None and need.get(sv[0], 0) < sv[1]:
                need[sv[0]] = sv[1]
        for r in reads:
            add(self.last_w.get(r))
        for w in writes:
            add(self.last_w.get(w))
            for rd in self.readers.get(w, ()):
                add(rd)
        waits = []
        seen = self.seen[eng]
        for k, v in need.items():
            if seen.get(k, 0) < v:
                seen[k] = v
                waits.append((k, v))
        return waits

    def _commit(self, reads, writes, me):
        for r in reads:
            self.readers.setdefault(r, []).append(me)
        for w in writes:
            self.last_w[w] = me
            self.readers[w] = []

    def _emit(self, eng, fn, waits, inc):
        e = getattr(self.nc, ENGATTR[eng])
        for (k, v) in waits:
            e.wait_ge(self._semh(k), v)
        if fn is not None:
            fn(e).then_inc(self._semh(inc[0]), inc[1])
        self.n_ops += 1

    def op(self, eng, fn, reads=(), writes=()):
        waits = self._deps(eng, reads, writes)
        self.cnt[eng] += 1
        self._emit(eng, fn, waits, (eng, 1))
        self._commit(reads, writes, (eng, self.cnt[eng]))

    def dma(self, q, chan, out, in_, reads=(), writes=(), **kw):
        if chan not in self.chan:
            used = set(self.chan.values())
            idx = None
            for i, ent in enumerate(self.cpool):
                if ent[2] == q and i not in used:
                    idx = i
                    break
            if idx is None:
                idx = len(self.cpool)
                self.cpool.append([self.es.enter_context(self.nc.semaphore("c_%d" % idx)), 0, q])
            self.chan[chan] = idx
        idx = self.chan[chan]
        ch = self.cpool[idx]
        waits = self._deps(q, reads, writes)
        if ch[1] > 0 and self.seen[q].get(idx, 0) < ch[1]:
            self.seen[q][idx] = ch[1]
            waits.append((idx, ch[1]))
        ch[1] += 16
        self._emit(q, lambda e: e.dma_start(out=out, in_=in_, **kw), waits, (idx, 16))
        self._commit(reads, writes, (idx, ch[1]))

    def barrier(self):
        nc = self.nc
        self.epoch += 1
        tot = {e: self.cnt[e] for e in ENGS if self.cnt[e] > 0}
        for idx, ent in enumerate(self.cpool):
            if ent[1] > 0:
                tot[idx] = ent[1]
        for e in ENGS:
            eng = getattr(nc, ENGATTR[e])
            for k, v in tot.items():
                if self.seen[e].get(k, 0) < v:
                    eng.wait_ge(self._semh(k), v)
            if e != "sp":
                eng.sem_inc(self.sem["arrive"], 1)
        sp = nc.sync
        sp.wait_ge(self.sem["arrive"], 4 * self.epoch)
        for k in tot:
            if k in ENGS:
                sp.sem_clear(self._semh(k))
        sp.drain().then_inc(self.sem["go"], 1)
        for e in ENGS:
            if e != "sp":
                getattr(nc, ENGATTR[e]).wait_ge(self.sem["go"], self.epoch)
        for e in ENGS:
            self.cnt[e] = 0
            self.seen[e] = {k: v for k, v in tot.items() if k not in ENGS}
        if RECYCLE_SEMS:
            self.chan = {}
        self.last_w.clear()
        self.readers.clear()

    def finish(self):
        self.barrier()
        self.es.close()


class Pool:
    def __init__(self, kb, tag):
        self.kb = kb
        self.tag = tag
        self.es = ExitStack()

    def sb(self, name, shape, dt=F32):
        self.kb.uid += 1
        t = self.es.enter_context(self.kb.nc.sbuf_tensor("%s_%s_%d" % (self.tag, name, self.kb.uid), list(shape), dt))
        return T(t, name)

    def ps(self, name, shape, dt=F32):
        self.kb.uid += 1
        t = self.es.enter_context(self.kb.nc.psum_tensor("%s_%s_%d" % (self.tag, name, self.kb.uid), list(shape), dt))
        return T(t, name)

    def close(self):
        self.kb.barrier()
        self.es.close()


def mm(kb, out, lhsT, rhs, start, stop, reads, writes, sgc=False):
    if sgc:
        kb.op("pe", lambda e: e.matmul(out, lhsT=lhsT, rhs=rhs, start=start, stop=stop, skip_group_check=True), reads=reads, writes=writes)
    else:
        kb.op("pe", lambda e: e.matmul(out, lhsT=lhsT, rhs=rhs, start=start, stop=stop), reads=reads, writes=writes)


def tr(kb, out, in_, ident, reads, writes):
    kb.op("pe", lambda e: e.transpose(out=out, in_=in_, identity=ident), reads=reads, writes=writes)


def act(kb, out, in_, func, reads, writes, **kw):
    kb.op("act", lambda e: e.activation(out=out, in_=in_, func=func, **kw), reads=reads, writes=writes)


def cp(kb, eng, out, in_, reads, writes):
    if eng == "act":
        act(kb, out, in_, AF.Copy, reads, writes)
    else:
        kb.op(eng, lambda e: e.tensor_copy(out=out, in_=in_), reads=reads, writes=writes)


def tt(kb, eng, out, in0, in1, op, reads, writes):
    kb.op(eng, lambda e: e.tensor_tensor(out=out, in0=in0, in1=in1, op=op), reads=reads, writes=writes)


def ts(kb, eng, out, in0, s1, op0, reads, writes, s2=None, op1=None):
    if op1 is None:
        kb.op(eng, lambda e: e.tensor_scalar(out=out, in0=in0, scalar1=s1, scalar2=None, op0=op0), reads=reads, writes=writes)
    else:
        kb.op(eng, lambda e: e.tensor_scalar(out=out, in0=in0, scalar1=s1, scalar2=s2, op0=op0, op1=op1), reads=reads, writes=writes)


def stt(kb, eng, out, in0, scalar, in1, op0, op1, reads, writes):
    kb.op(eng, lambda e: e.scalar_tensor_tensor(out=out, in0=in0, scalar=scalar, in1=in1, op0=op0, op1=op1), reads=reads, writes=writes)


class Ctx:
    pass


GDBG = {}


def load_w_bf16(kb, C, P, dst, dcol0, src, ncols, nk, stage_ring, cnt, pw=256):
    c = 0
    while c < ncols:
        w = min(pw, ncols - c)
        st = stage_ring[cnt[0] % len(stage_ring)]
        kb.dma("sp", st.k, st[:, 0:nk, 0:w], src[:, c:c + w].rearrange("(k p) n -> p k n", p=128), writes=[st.k])
        eng = ("pool", "dve")[cnt[0] % 2]
        cp(kb, eng, dst[:, 0:nk, dcol0 + c:dcol0 + c + w], st[:, 0:nk, 0:w], [st.k], [dst.k])
        cnt[0] += 1
        c += w


STAGE_IN = {
    "A": ["x_in", "cvec", "mod_w", "mod_bT", "w_in", "cw", "alog", "dtb", "cmat", "ropet"],
    "G0": ["gb_s", "kv_s", "qkgT_s", "onormg", "cmat"],
    "G1": ["gb_s", "kv_s", "qkgT_s", "oA_s", "zs_s", "onormg", "cmat"],
    "T": ["qT_s", "kT_s", "V_s", "lamv", "sublng", "lam_in", "cmat"],
    "M": ["modT_i", "x_in", "hT_s", "yaT_s", "ybT_s", "w_pa", "w_pb", "w_gate", "w_o", "lnp", "cmat"],
    "E": ["modT_i", "x1_s", "router_w", "router_b", "w_e1", "w_e3", "w_e2", "lnp", "cmat", "sel16"],
}
STAGE_OUT = {
    "A": ["modT_o", "hT_s", "qT_s", "kT_s", "V_s", "zs_s", "gb_s", "qkgT_s", "kv_s"],
    "G0": ["oA_s"],
    "G1": ["ybT_s"],
    "T": ["yaT_s"],
    "M": ["x1_s"],
    "E": ["xsA"],
}


def build_program(stage, dbg=(), stop_after=None):
    nc = bass.Bass("TRN2", target_bir_lowering=False)
    kb = KB(nc)
    C = Ctx()
    C.nc, C.kb = nc, kb
    NL = 1

    def din(name, shape, dt=F32):
        if name in STAGE_IN[stage]:
            kind = "ExternalInput"
        elif name in STAGE_OUT[stage] or name in dbg:
            kind = "ExternalOutput"
        else:
            kind = "Internal"
        return nc.dram_tensor(name, list(shape), dt, kind=kind).ap()

    dscr = din

    x_in = din("x_in", [TE if stage == "M" else TOK, D])
    cvec = din("cvec", [128, 8, 2])
    mod_w = din("mod_w", [NL, D, 6 * D])
    mod_bT = din("mod_bT", [NL, 128, 48])
    w_in = din("w_in", [NL, D, C_GATE])
    w_gate = din("w_gate", [D, 2048])
    cw_in = din("cw", [NL, 128, 12, 5])
    lamv = din("lamv", [NL, 4, 64])
    lam_in = din("lam_in", [128, 2])
    sublng = din("sublng", [NL, 128])
    onormg = din("onormg", [NL, 128])
    alog = din("alog", [NL, 8])
    dtb = din("dtb", [NL, 8])
    w_pa = din("w_pa", [NL, 512, D])
    w_pb = din("w_pb", [NL, 512, D])
    w_o = din("w_o", [NL, D, D])
    lnp = din("lnp", [NL, 4, D])
    router_w = din("router_w", [D, 16])
    router_b = din("router_b", [1, 16])
    w_e1 = din("w_e1", [NL, 16, D, 512])
    w_e3 = din("w_e3", [NL, 16, D, 512])
    w_e2 = din("w_e2", [NL, 16, 512, D])
    cmat_in = din("cmat", [128, 10, 128])
    ropet = din("ropet", [2, 128, S])
    sel16_in = din("sel16", [16, 16, 128])
    modT_i = din("modT_i", [128, 48, 2])
    modT_o = din("modT_o", [128, 48, 2])

    xsA = dscr("xsA", [TOK, D]) if stage != "E" else None
    xsB = dscr("xsB", [TOK, D])
    TOKE = TE if stage in ("E", "M") else TOK
    xsA = dscr("xsA", [TOKE, D]) if stage == "E" else xsA
    x1_s = dscr("x1_s", [TOKE, D])
    hT_s = dscr("hT_s", [128, 8, TOKE], BF16)
    qT_s = dscr("qT_s", [128, 4, QT if stage == "T" else TOK], BF16)
    kT_s = dscr("kT_s", [128, 4, TOK], BF16)
    V_s = dscr("V_s", [TOK, 4, VP], BF16)
    zs_s = dscr("zs_s", [TOK, 512])
    gb_s = dscr("gb_s", [TOK, 16])
    qkgT_s = dscr("qkgT_s", [128, 8, TOK])
    kv_s = dscr("kv_s", [TOK, 8, 128])
    oA_s = dscr("oA_s", [TOK, 512])
    ybT_s = dscr("ybT_s", [128, 4, TE if stage == "M" else TOK], BF16)
    yaT_s = dscr("yaT_s", [128, 4, QT if stage == "T" else (TE if stage == "M" else TOK)], BF16)
    wtT_s = dscr("wtT_s", [16, TOKE])
    dbg_mod = dbg_ymix = dbg_ymoe = dbg_oB = dbg_yb = dbg_ya = None
    out = None

    G = Pool(kb, "G")
    cmat = G.sb("cmat", [128, 10, 128])
    kb.dma("sp", "cmat", cmat[:], cmat_in, writes=[cmat.k])
    ident = cmat[:, 0, :]
    ones = cmat[:, 1, :]
    INCL = [cmat[:, 2, :], cmat[:, 3, :]]
    NEGM = [cmat[:, 4, :], cmat[:, 5, :]]
    STRICT = [cmat[:, 6, :], cmat[:, 7, :]]
    CHSEL = cmat[:, 8, 0:2]
    BLK = cmat[:, 9, :]
    CK = cmat.k
    modT = [G.sb("modT%d" % l, [128, 48, 2]) for l in range(NL)]
    epsT = G.sb("epsT", [128, 1])
    kb.op("dve", lambda e: e.memset(epsT[:], EPS), writes=[epsT.k])
    sel16 = G.sb("sel16", [16, 16, 128])
    rw = G.sb("rw", [128, 8, 16])
    rb_bc = G.sb("rb_bc", [128, 16])
    if stage == "E":
        kb.dma("sp", "sel16", sel16[:], sel16_in, writes=[sel16.k])
        kb.dma("sp", "rw", rw[:], router_w.rearrange("(k p) e -> p k e", p=128), writes=[rw.k], allow_slow_non_contiguous=True)
        kb.dma("sp", "rb_bc", rb_bc[:], router_b.partition_broadcast(128), writes=[rb_bc.k])

    P = Pool(kb, "pro")
    NLp = NL if stage == "A" else 0
    sT = P.sb("sT", [128, 8, 2])
    cv = P.sb("cv", [128, 8, 2])
    mbT = P.sb("mbT", [128, NL, 48])
    if stage == "A":
        kb.dma("sp", "cv", cv[:], cvec, writes=[cv.k])
        act(kb, sT[:], cv[:], AF.Silu, [cv.k], [sT.k])
        for l in range(NL):
            kb.dma("sp", "mbT", mbT[:, l, :], mod_bT[l], writes=[mbT.k])
    wst = [P.sb("wst%d" % i, [128, 8, 512]) for i in range(3)]
    pp = [P.ps("pp%d" % i, [128, 8]) for i in range(2)]
    it = 0
    for l in range(NLp):
        for cb in range(12):
            st = wst[it % 3]
            ps = pp[it % 2]
            kb.dma("sp", st.k, st[:], mod_w[l][:, cb * 512:(cb + 1) * 512].rearrange("(k p) n -> p k n", p=128), writes=[st.k])
            for fc in range(4):
                for k in range(8):
                    mm(kb, ps[:, fc * 2:fc * 2 + 2], st[:, k, fc * 128:(fc + 1) * 128], sT[:, k, :], k == 0, k == 7,
                       [st.k, sT.k], [ps.k])
            tt(kb, "dve", modT[l][:, cb * 4:cb * 4 + 4, :], ps[:].rearrange("p (c j) -> p c j", j=2),
               mbT[:, l, cb * 4:cb * 4 + 4].unsqueeze(2).to_broadcast([128, 4, 2]), ALU.add, [ps.k, mbT.k], [modT[l].k])
            it += 1
        if dbg_mod is not None:
            kb.dma("sp", "dbgmod", dbg_mod[l], modT[l][:], reads=[modT[l].k], writes=["dbgmod"])
    if stage == "A":
        kb.dma("sp", "modTo", modT_o, modT[0][:], reads=[modT[0].k], writes=["modTo"])
    elif stage in ("M", "E"):
        kb.dma("sp", "modTi", modT[0][:], modT_i, writes=[modT[0].k])
    P.close()

    xs_cur = x_in
    xs_bufs = [xsA, xsB]
    for l in range(NL):
        xs_next = xsA
        last = False

        LP = Pool(kb, "lc%d" % l)
        sc1 = LP.sb("sc1", [128, 8, 2])
        sc4 = LP.sb("sc4", [128, 8, 2])
        if stage in ("A", "M", "E"):
            ts(kb, "dve", sc1[:], modT[l][:, 8:16, :], 1.0, ALU.add, [modT[l].k], [sc1.k])
            ts(kb, "dve", sc4[:], modT[l][:, 32:40, :], 1.0, ALU.add, [modT[l].k], [sc4.k])
        sh0 = modT[l][:, 0:8, :]
        sh3 = modT[l][:, 24:32, :]
        gbc = {}
        for nm in ({"M": ("m2",), "E": ("m5",)}.get(stage, ())):
            for j in range(2):
                gbc[(nm, j)] = LP.sb("%s_%d" % (nm, j), [128, D])
        lnbc = LP.sb("lnbc", [128, 4, D])
        Pg = Pool(kb, "gbc%d" % l)
        _do_gbc = stage in ("M", "E")
        dg = [Pg.sb("dg%d" % i, [128, 128]) for i in range(2)]
        pg = [Pg.ps("pg%d" % i, [128, 512]) for i in range(2)]
        n = 0
        for (nm, c0) in ({"M": (("m2", 16),), "E": (("m5", 40),)}.get(stage, ())):
            for j in range(2):
                t = gbc[(nm, j)]
                for half in range(2):
                    ps = pg[n % 2]
                    for kk in range(4):
                        k = half * 4 + kk
                        d_ = dg[(n * 4 + kk) % 2]
                        ts(kb, "dve", d_[:], ident, modT[l][:, c0 + k, j:j + 1], ALU.mult, [CK, modT[l].k], [d_.k])
                        mm(kb, ps[:, kk * 128:(kk + 1) * 128], ones, d_[:], True, True, [CK, d_.k], [ps.k])
                    cp(kb, "act", t[:, half * 512:(half + 1) * 512], ps[:], [ps.k], [t.k])
                    n += 1
        Pg.close()
        for i in (range(4) if _do_gbc else ()):
            kb.dma("sp", "lnbc%d" % i, lnbc[:, i, :], lnp[l][i:i + 1, :].partition_broadcast(128), writes=[lnbc.k + str(i)])
        LNK = [lnbc.k + str(i) for i in range(4)]

        def make_hT(tag, x_src, scale, shift, hT_dst, want_router=False, blocks=BLOCKS):
            Pa = Pool(kb, tag)
            xt = [Pa.sb("xt%d" % i, [128, D]) for i in range(8)]
            ptr = [Pa.ps("ptr%d" % i, [128, 512]) for i in range(4)]
            hb = [Pa.sb("hb%d" % i, [128, 8, 512], BF16) for i in range(2)]
            h32 = [Pa.sb("h32_%d" % i, [128, 8, 512]) for i in range(2)] if want_router else None
            prl = [Pa.ps("prl%d" % i, [128, 16]) for i in range(2)] if want_router else None
            ptw = Pa.ps("ptw", [16, 128]) if want_router else None
            rt = {}
            if want_router:
                for nm, shp in (("sc", [128, 16]), ("sel", [128, 16]), ("m1", [128, 4]), ("ge", [128, 16]), ("s2", [128, 16]),
                                ("m2", [128, 4]), ("gs", [128, 4]), ("gm", [128, 1]), ("gmask", [128, 4]), ("selm", [128, 16]),
                                ("t1", [128, 1]), ("k1", [128, 16]), ("selm2", [128, 16]), ("t2", [128, 1]), ("k2", [128, 16]),
                                ("ch", [128, 16]), ("w", [128, 16]), ("ws", [128, 1]), ("wr", [128, 1]), ("wt", [128, 16]),
                                ("wtT", [16, 128])):
                    rt[nm] = [Pa.sb("r_%s%d" % (nm, i), shp) for i in range(2)]
            xi = 0
            for bi, (t0, bs, isc) in enumerate(blocks):
                j = 1 if isc else 0
                ntl = bs // 128
                tiles = []
                for q in range(ntl):
                    x_ = xt[xi % 8]
                    xi += 1
                    kb.dma("sp", x_.k, x_[:], x_src[t0 + q * 128:t0 + (q + 1) * 128, :], writes=[x_.k])
                    tiles.append(x_)
                h_ = hb[bi % 2]
                for k in range(8):
                    ps = ptr[k % 4]
                    for q in range(ntl):
                        tr(kb, ps[:, q * 128:(q + 1) * 128], tiles[q][:, k * 128:(k + 1) * 128], ident, [tiles[q].k, CK], [ps.k])
                    act(kb, h_[:, k, 0:bs], ps[:, 0:bs], AF.Identity, [ps.k, scale.k, shift.k], [h_.k],
                        scale=scale[:, k, j:j + 1], bias=shift[:, k, j:j + 1])
                    if want_router:
                        h3 = h32[bi % 2]
                        act(kb, h3[:, k, 0:bs], ps[:, 0:bs], AF.Identity, [ps.k, scale.k, shift.k], [h3.k],
                            scale=scale[:, k, j:j + 1], bias=shift[:, k, j:j + 1])
                kb.dma("pool", "st_" + h_.k, hT_dst[:, :, t0:t0 + bs], h_[:, :, 0:bs], reads=[h_.k], writes=["hTdst"])
                if want_router:
                    h3 = h32[bi % 2]
                    for q in range(ntl):
                        ti = (t0 // 128) + q
                        r = {nm: v[ti % 2] for nm, v in rt.items()}
                        pl = prl[ti % 2]
                        for k in range(8):
                            mm(kb, pl[:], h3[:, k, q * 128:(q + 1) * 128], rw[:, k, :], k == 0, k == 7, [h3.k, rw.k], [pl.k])
                        act(kb, r["sc"][:], pl[:], AF.Sigmoid, [pl.k], [r["sc"].k])
                        tt(kb, "dve", r["sel"][:], r["sc"][:], rb_bc[:], ALU.add, [r["sc"].k, rb_bc.k], [r["sel"].k])
                        if GDBG.get("rstage", 99) < 1:
                            continue
                        sel3 = r["sel"][:].rearrange("p (g k) -> p g k", g=4)
                        kb.op("dve", lambda e, o=r["m1"][:], i=sel3: e.tensor_reduce(out=o, in_=i, axis=AX.X, op=ALU.max),
                              reads=[r["sel"].k], writes=[r["m1"].k])
                        tt(kb, "dve", r["ge"][:].rearrange("p (g k) -> p g k", g=4), sel3,
                           r["m1"][:].unsqueeze(2).to_broadcast([128, 4, 4]), ALU.is_ge, [r["sel"].k, r["m1"].k], [r["ge"].k])
                        stt(kb, "dve", r["s2"][:], r["ge"][:], NEGBIG, r["sel"][:], ALU.mult, ALU.add, [r["ge"].k, r["sel"].k], [r["s2"].k])
                        kb.op("dve", lambda e, o=r["m2"][:], i=r["s2"][:].rearrange("p (g k) -> p g k", g=4):
                              e.tensor_reduce(out=o, in_=i, axis=AX.X, op=ALU.max), reads=[r["s2"].k], writes=[r["m2"].k])
                        tt(kb, "dve", r["gs"][:], r["m1"][:], r["m2"][:], ALU.add, [r["m1"].k, r["m2"].k], [r["gs"].k])
                        kb.op("dve", lambda e, o=r["gm"][:], i=r["gs"][:]: e.tensor_reduce(out=o, in_=i, axis=AX.X, op=ALU.max),
                              reads=[r["gs"].k], writes=[r["gm"].k])
                        ts(kb, "dve", r["gmask"][:], r["gs"][:], r["gm"][:, 0:1], ALU.is_ge, [r["gs"].k, r["gm"].k], [r["gmask"].k])
                        if GDBG.get("rstage", 99) < 2:
                            continue
                        ts(kb, "dve", r["gs"][:], r["gmask"][:], -1.0, ALU.add, [r["gmask"].k], [r["gs"].k], s2=-NEGBIG, op1=ALU.mult)
                        tt(kb, "dve", r["selm"][:].rearrange("p (g k) -> p g k", g=4), sel3,
                           r["gs"][:].unsqueeze(2).to_broadcast([128, 4, 4]), ALU.add, [r["sel"].k, r["gs"].k], [r["selm"].k])
                        kb.op("dve", lambda e, o=r["t1"][:], i=r["selm"][:]: e.tensor_reduce(out=o, in_=i, axis=AX.X, op=ALU.max),
                              reads=[r["selm"].k], writes=[r["t1"].k])
                        ts(kb, "dve", r["k1"][:], r["selm"][:], r["t1"][:, 0:1], ALU.is_ge, [r["selm"].k, r["t1"].k], [r["k1"].k])
                        stt(kb, "dve", r["selm2"][:], r["k1"][:], NEGBIG, r["selm"][:], ALU.mult, ALU.add, [r["k1"].k, r["selm"].k], [r["selm2"].k])
                        kb.op("dve", lambda e, o=r["t2"][:], i=r["selm2"][:]: e.tensor_reduce(out=o, in_=i, axis=AX.X, op=ALU.max),
                              reads=[r["selm2"].k], writes=[r["t2"].k])
                        ts(kb, "dve", r["k2"][:], r["selm2"][:], r["t2"][:, 0:1], ALU.is_ge, [r["selm2"].k, r["t2"].k], [r["k2"].k])
                        tt(kb, "dve", r["ch"][:], r["k1"][:], r["k2"][:], ALU.add, [r["k1"].k, r["k2"].k], [r["ch"].k])
                        if GDBG.get("rstage", 99) < 3:
                            continue
                        tt(kb, "dve", r["w"][:], r["ch"][:], r["sc"][:], ALU.mult, [r["ch"].k, r["sc"].k], [r["w"].k])
                        kb.op("dve", lambda e, o=r["ws"][:]: e.memset(o, 0.0), writes=[r["ws"].k])
                        act(kb, r["selm2"][:], r["w"][:], AF.Identity, [r["w"].k], [r["selm2"].k, r["ws"].k], accum_out=r["ws"][:])
                        kb.op("dve", lambda e, o=r["wr"][:], i=r["ws"][:]: e.reciprocal(out=o, in_=i), reads=[r["ws"].k], writes=[r["wr"].k])
                        ts(kb, "dve", r["wt"][:], r["w"][:], r["wr"][:, 0:1], ALU.mult, [r["w"].k, r["wr"].k], [r["wt"].k])
                        if GDBG.get("rstage", 99) < 4:
                            continue
                        mm(kb, ptw[:], r["wt"][:], ident, True, True, [r["wt"].k, CK], [ptw.k])
                        cp(kb, "act", r["wtT"][:], ptw[:], [ptw.k], [r["wtT"].k])
                        kb.dma("pool", "st_wtT%d" % (ti % 2), wtT_s[:, ti * 128:(ti + 1) * 128], r["wtT"][:], reads=[r["wtT"].k], writes=["wtTdst"])
            Pa.close()

        if stage == "A":
            make_hT("A1_%d" % l, xs_cur, sc1, modT[l], hT_s)
        if stop_after == "A1":
            break

        for part in (("a", "b") if stage == "A" else ()):
            Pa = Pool(kb, "A2%s_%d" % (part, l))
            wstage = [Pa.sb("wstg%d" % i, [128, 8, 256]) for i in range(2)]
            wcnt = [0]
            hw = [Pa.sb("hw%d" % i, [128, 8, 516], BF16) for i in range(2)]
            pH = Pa.ps("pH", [128, 16])
            if part == "a":
                Wqk = Pa.sb("Wqk", [128, 8, 2048], BF16)
                Wv = Pa.sb("Wv", [128, 8, 512], BF16)
                Wz = Pa.sb("Wz", [128, 8, 512], BF16)
                Wab = Pa.sb("Wab", [128, 8, 16], BF16)
                load_w_bf16(kb, C, Pa, Wqk, 0, w_in[l][:, C_QA:C_QA + 2048], 2048, 8, wstage, wcnt)
                load_w_bf16(kb, C, Pa, Wv, 0, w_in[l][:, C_VA:C_VA + 512], 512, 8, wstage, wcnt)
                load_w_bf16(kb, C, Pa, Wz, 0, w_in[l][:, C_Z:C_Z + 512], 512, 8, wstage, wcnt)
                load_w_bf16(kb, C, Pa, Wab, 0, w_in[l][:, C_AB:C_AB + 16], 16, 8, wstage, wcnt)
                dtb_bc = Pa.sb("dtb_bc", [128, 8])
                kb.dma("sp", "dtb_bc", dtb_bc[:], dtb[l:l + 1, :].partition_broadcast(128), writes=[dtb_bc.k])
                nea = Pa.sb("nea", [128, 8])
                kb.dma("sp", "nea", nea[:], alog[l:l + 1, :].partition_broadcast(128), writes=[nea.k])
                act(kb, nea[:], nea[:], AF.Exp, [nea.k], [nea.k])
                ts(kb, "dve", nea[:], nea[:], -1.0, ALU.mult, [nea.k], [nea.k])
                cosb = [Pa.sb("cos%d" % i, [128, 512]) for i in range(2)]
                sinb = [Pa.sb("sin%d" % i, [128, 512]) for i in range(2)]
                qko = [Pa.sb("qko%d" % i, [128, 4, 512], BF16) for i in range(2)]
                rt1 = [Pa.sb("rt1_%d" % i, [128, 512]) for i in range(2)]
                rt2 = [Pa.sb("rt2_%d" % i, [128, 512]) for i in range(2)]
                vt = [Pa.sb("vt%d" % i, [128, 4, VP], BF16) for i in range(2)]
                for v_ in vt:
                    kb.op("dve", lambda e, v_=v_: e.memset(v_[:], 1.0), writes=[v_.k])
                zt = [Pa.sb("zt%d" % i, [128, 512]) for i in range(2)]
                gbt = [Pa.sb("gbt%d" % i, [128, 16]) for i in range(2)]
                abt = [Pa.sb("abt%d" % i, [128, 8]) for i in range(2)]
                pA = [Pa.ps("pA%d" % i, [128, 512]) for i in range(2)]
                pB = [Pa.ps("pB%d" % i, [128, 512]) for i in range(2)]
                pTok = [Pa.ps("pTok%d" % i, [128, 512]) for i in range(2)]
            else:
                Wg = Pa.sb("Wg", [128, 8, 1536], BF16)
                load_w_bf16(kb, C, Pa, Wg, 0, w_in[l][:, C_GQ:C_GQ + 1536], 1536, 8, wstage, wcnt)
                cwt = Pa.sb("cwt", [128, 12, 5])
                kb.dma("sp", "cwt", cwt[:], cw_in[l], writes=[cwt.k])
                stg = [Pa.sb("stg%d" % i, [128, 516]) for i in range(2)]
                acc = [Pa.sb("acc%d" % i, [128, 512]) for i in range(2)]
                post = [Pa.sb("post%d" % i, [128, 512]) for i in range(2)]
                sq = [Pa.sb("sq%d" % i, [128, 512]) for i in range(2)]
                rs = [Pa.sb("rs%d" % i, [128, 512]) for i in range(2)]
                gT = [Pa.sb("gT%d" % i, [128, 12, 512]) for i in range(2)]
                kvt = [Pa.sb("kvt%d" % i, [128, 8, 128]) for i in range(2)]
                pM = [Pa.ps("pM%d" % i, [128, 512]) for i in range(3)]
                pSS = [Pa.ps("pSS%d" % i, [128, 512]) for i in range(2)]
                pTr = [Pa.ps("pTr%d" % i, [128, 512]) for i in range(2)]
            cnt = {"qk": 0, "tile": 0, "cb": 0, "h": 0, "tr": 0}
            for bi, (t0, bs, isc) in enumerate(BLOCKS):
                ntl = bs // 128
                h_ = hw[bi % 2]
                kb.dma("sp", h_.k, h_[:, :, 2:2 + bs], hT_s[:, :, t0:t0 + bs], reads=["hTdst"], writes=[h_.k])
                if part == "b":
                    if t0 == 0 or t0 == S:
                        kb.op("pool", lambda e, h_=h_: e.memset(h_[:, :, 0:2], 0.0), writes=[h_.k + "L"])
                    else:
                        kb.dma("sp", h_.k + "L", h_[:, :, 0:2], hT_s[:, :, t0 - 2:t0], reads=["hTdst"], writes=[h_.k + "L"])
                    if t0 + bs == S or t0 + bs == TOK:
                        kb.op("pool", lambda e, h_=h_, bs=bs: e.memset(h_[:, :, bs + 2:bs + 4], 0.0), writes=[h_.k + "R"])
                    else:
                        kb.dma("sp", h_.k + "R", h_[:, :, bs + 2:bs + 4], hT_s[:, :, t0 + bs:t0 + bs + 2], reads=["hTdst"], writes=[h_.k + "R"])
                if part == "a":
                    if not isc:
                        cb_, sb_ = cosb[bi % 2], sinb[bi % 2]
                        kb.dma("sp", cb_.k, cb_[:, 0:bs], ropet[0][:, t0:t0 + bs], writes=[cb_.k])
                        kb.dma("sp", sb_.k, sb_[:, 0:bs], ropet[1][:, t0:t0 + bs], writes=[sb_.k])
                    for gi, (c0, dst) in enumerate(((0, qT_s), (1024, kT_s))):
                        o_ = qko[cnt["qk"] % 2]
                        cnt["qk"] += 1
                        for h in range(4):
                            pa_, pb_ = pA[cnt["h"] % 2], pB[cnt["h"] % 2]
                            cnt["h"] += 1
                            for k in range(8):
                                mm(kb, pa_[:, 0:bs], Wqk[:, k, c0 + h * 128:c0 + (h + 1) * 128], h_[:, k, 2:2 + bs], k == 0, k == 7,
                                   [Wqk.k, h_.k], [pa_.k])
                            if isc:
                                cp(kb, "act", o_[:, h, 0:bs], pa_[:, 0:bs], [pa_.k], [o_.k])
                            else:
                                for k in range(8):
                                    mm(kb, pb_[:, 0:bs], Wqk[:, k, c0 + 512 + h * 128:c0 + 512 + (h + 1) * 128], h_[:, k, 2:2 + bs], k == 0, k == 7,
                                       [Wqk.k, h_.k], [pb_.k])
                                a1, a2 = rt1[h % 2], rt2[h % 2]
                                tt(kb, "dve", a1[:, 0:bs], pa_[:, 0:bs], cb_[:, 0:bs], ALU.mult, [pa_.k, cb_.k], [a1.k])
                                tt(kb, "dve", a2[:, 0:bs], pb_[:, 0:bs], sb_[:, 0:bs], ALU.mult, [pb_.k, sb_.k], [a2.k])
                                tt(kb, "pool", o_[:, h, 0:bs], a1[:, 0:bs], a2[:, 0:bs], ALU.add, [a1.k, a2.k], [o_.k])
                        kb.dma("pool", "st_" + o_.k, dst[:, :, t0:t0 + bs], o_[:, :, 0:bs], reads=[o_.k], writes=["qkdst%d" % gi])
                    for q in range(ntl):
                        ti = cnt["tile"]
                        cnt["tile"] += 1
                        lt = slice(2 + q * 128, 2 + (q + 1) * 128)
                        tok = slice(t0 + q * 128, t0 + (q + 1) * 128)
                        v_ = vt[ti % 2]
                        pt0, pt1 = pTok[0], pTok[1]
                        for k in range(8):
                            mm(kb, pt0[:], h_[:, k, lt], Wv[:, k, :], k == 0, k == 7, [h_.k, Wv.k], [pt0.k])
                        cp(kb, "act", v_[:, :, 0:128], pt0[:].rearrange("p (h e) -> p h e", h=4), [pt0.k], [v_.k])
                        kb.dma("pool", "st_" + v_.k, V_s[tok, :, :], v_[:], reads=[v_.k], writes=["Vdst"])
                        z_ = zt[ti % 2]
                        for k in range(8):
                            mm(kb, pt1[:], h_[:, k, lt], Wz[:, k, :], k == 0, k == 7, [h_.k, Wz.k], [pt1.k])
                        act(kb, z_[:], pt1[:], AF.Silu, [pt1.k], [z_.k])
                        kb.dma("pool", "st_" + z_.k, zs_s[tok, :], z_[:], reads=[z_.k], writes=["zsdst"])
                        g_ = gbt[ti % 2]
                        a_ = abt[ti % 2]
                        for k in range(8):
                            mm(kb, pH[:, 0:16], h_[:, k, lt], Wab[:, k, :], k == 0, k == 7, [h_.k, Wab.k], [pH.k])
                        tt(kb, "dve", a_[:], pH[:, 0:8], dtb_bc[:], ALU.add, [pH.k, dtb_bc.k], [a_.k])
                        act(kb, a_[:], a_[:], AF.Exp, [a_.k], [a_.k])
                        act(kb, a_[:], a_[:], AF.Ln, [a_.k], [a_.k], bias=1.0)
                        tt(kb, "dve", g_[:, 0:8], a_[:], nea[:], ALU.mult, [a_.k, nea.k], [g_.k])
                        act(kb, g_[:, 8:16], pH[:, 8:16], AF.Sigmoid, [pH.k], [g_.k])
                        kb.dma("pool", "st_" + g_.k, gb_s[tok, :], g_[:], reads=[g_.k], writes=["gbdst"])
                    continue
                gT_ = gT[bi % 2]
                for cbi in range(12):
                    ci = cnt["cb"]
                    cnt["cb"] += 1
                    pm = pM[ci % 3]
                    for k in range(8):
                        mm(kb, pm[:, 0:bs], Wg[:, k, cbi * 128:(cbi + 1) * 128], h_[:, k, 2:2 + bs], k == 0, k == 7, [Wg.k, h_.k], [pm.k])
                    for k in range(8):
                        mm(kb, pH[:, 0:2], Wg[:, k, cbi * 128:(cbi + 1) * 128], h_[:, k, 0:2], k == 0, k == 7, [Wg.k, h_.k + "L"], [pH.k])
                    for k in range(8):
                        mm(kb, pH[:, 2:4], Wg[:, k, cbi * 128:(cbi + 1) * 128], h_[:, k, bs + 2:bs + 4], k == 0, k == 7, [Wg.k, h_.k + "R"], [pH.k])
                    s_ = stg[ci % 2]
                    cp(kb, "act", s_[:, 2:2 + bs], pm[:, 0:bs], [pm.k], [s_.k])
                    cp(kb, "dve", s_[:, 0:2], pH[:, 0:2], [pH.k], [s_.k])
                    cp(kb, "dve", s_[:, bs + 2:bs + 4], pH[:, 2:4], [pH.k], [s_.k])
                    ac = acc[ci % 2]
                    ts(kb, "pool", ac[:, 0:bs], s_[:, 0:bs], cwt[:, cbi, 0:1], ALU.mult, [s_.k, cwt.k], [ac.k])
                    for kk in range(1, 5):
                        stt(kb, "dve", ac[:, 0:bs], s_[:, kk:kk + bs], cwt[:, cbi, kk:kk + 1], ac[:, 0:bs], ALU.mult, ALU.add,
                            [s_.k, cwt.k, ac.k], [ac.k])
                    if cbi >= 8:
                        act(kb, gT_[:, cbi, 0:bs], ac[:, 0:bs], AF.Silu, [ac.k], [gT_.k])
                    else:
                        po = post[ci % 2]
                        act(kb, po[:, 0:bs], ac[:, 0:bs], AF.Silu, [ac.k], [po.k])
                        sq_ = sq[ci % 2]
                        tt(kb, "dve", sq_[:, 0:bs], po[:, 0:bs], po[:, 0:bs], ALU.mult, [po.k], [sq_.k])
                        pss = pSS[ci % 2]
                        mm(kb, pss[:, 0:bs], ones, sq_[:, 0:bs], True, True, [CK, sq_.k], [pss.k])
                        r_ = rs[ci % 2]
                        act(kb, r_[:, 0:bs], pss[:, 0:bs], AF.Sqrt, [pss.k, epsT.k], [r_.k], bias=epsT[:, 0:1], scale=1.0)
                        kb.op("dve", lambda e, o=r_[:, 0:bs]: e.reciprocal(out=o, in_=o), reads=[r_.k], writes=[r_.k])
                        if cbi < 4:
                            stt(kb, "dve", gT_[:, cbi, 0:bs], po[:, 0:bs], 128.0 ** -0.5, r_[:, 0:bs], ALU.mult, ALU.mult, [po.k, r_.k], [gT_.k])
                        else:
                            tt(kb, "dve", gT_[:, cbi, 0:bs], po[:, 0:bs], r_[:, 0:bs], ALU.mult, [po.k, r_.k], [gT_.k])
                kb.dma("pool", "st_" + gT_.k, qkgT_s[:, :, t0:t0 + bs], gT_[:, 0:8, 0:bs], reads=[gT_.k], writes=["qkgdst"])
                for q in range(ntl):
                    kv_ = kvt[q % 2]
                    for half in range(2):
                        ptr_ = pTr[cnt["tr"] % 2]
                        cnt["tr"] += 1
                        for i in range(4):
                            tr(kb, ptr_[:, i * 128:(i + 1) * 128], gT_[:, 4 + half * 4 + i, q * 128:(q + 1) * 128], ident, [gT_.k, CK], [ptr_.k])
                        cp(kb, ("act", "dve")[half], kv_[:, half * 4:(half + 1) * 4, :], ptr_[:].rearrange("p (h e) -> p h e", h=4), [ptr_.k], [kv_.k])
                    kb.dma("pool", "st_" + kv_.k, kv_s[t0 + q * 128:t0 + (q + 1) * 128, :, :], kv_[:], reads=[kv_.k], writes=["kvdst"])
            Pa.close()
        if stop_after == "A2":
            break

        for dr in ({"G0": (0,), "G1": (1,)}.get(stage, ())):
            Pg = Pool(kb, "G%d_%d" % (dr, l))
            Sst = Pg.sb("Sst", [128, 4, 128])
            kb.op("dve", lambda e: e.memset(Sst[:], 0.0), writes=[Sst.k + "0", Sst.k + "1", Sst.k + "2", Sst.k + "3"])
            on_bc = Pg.sb("on_bc", [128, 128])
            kb.dma("sp", "on_bc", on_bc[:], onormg[l:l + 1, :].partition_broadcast(128), writes=[on_bc.k])
            R2 = lambda nm, shp, dt=F32, n=2: [Pg.sb("%s%d" % (nm, i), shp, dt) for i in range(n)]
            gbl = R2("gbl", [128, 16])
            kvl = R2("kvl", [128, 8, 128])
            qkl = R2("qkl", [128, 8, 128])
            oAl = R2("oAl", [128, 512])
            zsl = R2("zsl", [128, 512])
            Y = R2("Y", [128, 4, 2])
            smS = R2("smS", [128, 16])
            egc = R2("egc", [128, 4])
            ngc = R2("ngc", [128, 4])
            kds = R2("kds", [128, 4])
            GLe = R2("GLe", [128, 8])
            nb4 = R2("nb4", [128, 4])
            ot = R2("ot", [128, 512])
            ybt = R2("ybt", [128, 512])
            ybT = R2("ybT", [128, 4, 128], BF16)
            ssq = R2("ssq", [128, 4])
            junk = R2("junk", [128, 128])
            H = {}
            for nm in ("diag", "pre", "DTi", "Ebc", "gs", "QKm", "Qa", "Qb", "QTa", "QTb", "TTa", "TTb", "kE", "wT", "ub", "qdT", "kdec", "vn"):
                H[nm] = [Pg.sb("%s_h%d" % (nm, h), [128, 128]) for h in range(4)]
            banks = [Pg.ps("bank%d" % i, [128, 512]) for i in range(8)]
            ph = {}
            for bi_, nm in enumerate(("gr", "qk", "rp", "x1", "x2", "ob", "sb")):
                ph[nm] = [T(banks[bi_].t[:, h * 128:(h + 1) * 128], "pbank_" + nm) for h in range(4)]
            psm = T(banks[7].t[:, 0:16], "psm")
            pws = ph["rp"]
            pob = ph["ob"]
            psb = ph["sb"]
            order = ([32, 33] + list(range(32))) if dr == 0 else ([33, 32] + list(range(31, -1, -1)))
            if GDBG.get("ntiles"):
                nt_ = GDBG["ntiles"]
                order = order[:(nt_[dr] if isinstance(nt_, list) else nt_)]
            for it_, ti in enumerate(order):
                tok = slice(ti * 128, (ti + 1) * 128)
                i2 = it_ % 2
                gb_, kv_, qk_ = gbl[i2], kvl[i2], qkl[i2]
                kb.dma("sp", gb_.k, gb_[:], gb_s[tok, :], reads=["gbdst"], writes=[gb_.k])
                kb.dma("sp", kv_.k, kv_[:], kv_s[tok, :, :], reads=["kvdst"], writes=[kv_.k])
                kb.dma("sp", qk_.k, qk_[:], qkgT_s[:, :, tok], reads=["qkgdst"], writes=[qk_.k])
                if dr == 1:
                    oa_, zs_ = oAl[i2], zsl[i2]
                    kb.dma("sp", oa_.k, oa_[:], oA_s[tok, :], reads=["oAdst"], writes=[oa_.k])
                    kb.dma("sp", zs_.k, zs_[:], zs_s[tok, :], reads=["zsdst"], writes=[zs_.k])
                if GDBG.get("zero_gb"):
                    kb.op("dve", lambda e, o=gb_[:]: e.memset(o, 0.0), writes=[gb_.k])
                if GDBG.get("zero_qk"):
                    kb.op("dve", lambda e, o=qk_[:]: e.memset(o, 0.0), writes=[qk_.k])
                g4 = gb_[:, dr * 4:dr * 4 + 4]
                b4 = gb_[:, 8 + dr * 4:8 + dr * 4 + 4]
                if GDBG.get("gstage", 99) < 0:
                    continue
                Y_, sm_, eg_, ng_, kd_, GL_, nb_ = Y[i2], smS[i2], egc[i2], ngc[i2], kds[i2], GLe[i2], nb4[i2]
                for c_ in range(2):
                    ts(kb, "dve", Y_[:, :, c_], g4, CHSEL[:, c_:c_ + 1], ALU.mult, [gb_.k, CK], [Y_.k])
                mm(kb, psm[:, 0:4], INCL[dr], g4, True, True, [CK, gb_.k], [psm.k])
                mm(kb, psm[:, 4:12], ones, Y_[:].rearrange("p h c -> p (h c)"), True, True, [CK, Y_.k], [psm.k])
                mm(kb, psm[:, 12:16], BLK, g4, True, True, [CK, gb_.k], [psm.k])
                cp(kb, "dve", sm_[:], psm[:], [psm.k], [sm_.k])
                if GDBG.get("gstage", 99) < 0.5:
                    continue
                act(kb, eg_[:], sm_[:, 0:4], AF.Exp, [sm_.k], [eg_.k])
                ts(kb, "dve", ng_[:], sm_[:, 0:4], -1.0, ALU.mult, [sm_.k], [ng_.k])
                tt(kb, "dve", kd_[:], sm_[:, 12:16], sm_[:, 0:4], ALU.subtract, [sm_.k], [kd_.k])
                act(kb, kd_[:], kd_[:], AF.Exp, [kd_.k], [kd_.k])
                act(kb, GL_[:], sm_[:, 4:12], AF.Exp, [sm_.k], [GL_.k])
                ts(kb, "dve", nb_[:], b4, -1.0, ALU.mult, [gb_.k], [nb_.k])
                if GDBG.get("gstage", 99) < 1:
                    continue
                HS = range(4)
                kT = lambda h: qk_[:, 4 + h, :]
                qT = lambda h: qk_[:, h, :]
                ktm = lambda h: kv_[:, h, :]
                vtm = lambda h: kv_[:, 4 + h, :]
                sk_ = GDBG.get("g2skip", "")
                for h in HS:
                    if "gram" not in sk_:
                        mm(kb, ph["gr"][h][:], kT(h), kT(h), True, True, [qk_.k], [ph["gr"][h].k])
                    if "qk" not in sk_:
                        mm(kb, ph["qk"][h][:], kT(h), qT(h), True, True, [qk_.k], [ph["qk"][h].k])
                    if "diag" not in sk_:
                        ts(kb, "dve", H["diag"][h][:], ident, sm_[:, h:h + 1], ALU.mult, [CK, sm_.k], [H["diag"][h].k])
                    if "rp" not in sk_:
                        mm(kb, ph["rp"][h][:], ones, H["diag"][h][:], True, True, [CK, H["diag"][h].k], [ph["rp"][h].k])
                if GDBG.get("gstage", 99) < 2:
                    continue
                for h in HS:
                    stt(kb, "dve", H["pre"][h][:], ph["rp"][h][:], ng_[:, h:h + 1], NEGM[dr], ALU.add, ALU.add,
                        [ph["rp"][h].k, ng_.k, CK], [H["pre"][h].k])
                    act(kb, H["DTi"][h][:], H["pre"][h][:], AF.Exp, [H["pre"][h].k], [H["DTi"][h].k])
                    act(kb, H["Ebc"][h][:], ph["rp"][h][:], AF.Exp, [ph["rp"][h].k], [H["Ebc"][h].k])
                    tt(kb, "dve", H["gs"][h][:], ph["gr"][h][:], STRICT[dr], ALU.mult, [ph["gr"][h].k, CK], [H["gs"][h].k])
                if GDBG.get("gstage", 99) < 3:
                    continue
                for h in HS:
                    stt(kb, "dve", H["Qa"][h][:], H["gs"][h][:], nb_[:, h:h + 1], H["DTi"][h][:], ALU.mult, ALU.mult,
                        [H["gs"][h].k, nb_.k, H["DTi"][h].k], [H["Qa"][h].k])
                    tt(kb, "dve", H["QKm"][h][:], ph["qk"][h][:], H["DTi"][h][:], ALU.mult, [ph["qk"][h].k, H["DTi"][h].k], [H["QKm"][h].k])
                    tr(kb, ph["x1"][h][:], H["Qa"][h][:], ident, [H["Qa"][h].k, CK], [ph["x1"][h].k])
                    tt(kb, "dve", H["TTa"][h][:], H["Qa"][h][:], ident, ALU.add, [H["Qa"][h].k, CK], [H["TTa"][h].k])
                    tt(kb, "dve", H["qdT"][h][:], qT(h), H["Ebc"][h][:], ALU.mult, [qk_.k, H["Ebc"][h].k], [H["qdT"][h].k])
                    act(kb, H["kE"][h][:], ktm(h), AF.Copy, [kv_.k, eg_.k], [H["kE"][h].k], scale=eg_[:, h:h + 1])
                    act(kb, H["kdec"][h][:], ktm(h), AF.Copy, [kv_.k, kd_.k], [H["kdec"][h].k], scale=kd_[:, h:h + 1])
                for h in HS:
                    cp(kb, "act", H["QTa"][h][:], ph["x1"][h][:], [ph["x1"][h].k], [H["QTa"][h].k])
                if GDBG.get("gstage", 99) < 4:
                    continue
                Qc, QTc, TTc = "Qa", "QTa", "TTa"
                Qn, QTn, TTn = "Qb", "QTb", "TTb"
                for step in range(1, 6):
                    for h in HS:
                        mm(kb, ph["x1"][h][:], H[Qc][h][:], H[QTc][h][:], True, True, [H[Qc][h].k, H[QTc][h].k], [ph["x1"][h].k])
                        if step < 5:
                            mm(kb, ph["x2"][h][:], H[QTc][h][:], H[Qc][h][:], True, True, [H[Qc][h].k, H[QTc][h].k], [ph["x2"][h].k])
                    for h in HS:
                        cp(kb, "act", H[QTn][h][:], ph["x1"][h][:], [ph["x1"][h].k], [H[QTn][h].k])
                        if step < 5:
                            cp(kb, "dve", H[Qn][h][:], ph["x2"][h][:], [ph["x2"][h].k], [H[Qn][h].k])
                    for h in HS:
                        mm(kb, ph["x1"][h][:], H[QTn][h][:], H[TTc][h][:], True, True, [H[QTn][h].k, H[TTc][h].k], [ph["x1"][h].k])
                    for h in HS:
                        tt(kb, "dve", H[TTn][h][:], ph["x1"][h][:], H[TTc][h][:], ALU.add, [ph["x1"][h].k, H[TTc][h].k], [H[TTn][h].k])
                    Qc, Qn = Qn, Qc
                    QTc, QTn = QTn, QTc
                    TTc, TTn = TTn, TTc
                if GDBG.get("gstage", 99) < 5:
                    continue
                for h in HS:
                    TT_ = H[TTc][h]
                    mm(kb, ph["x1"][h][:], H["kE"][h][:], TT_[:], True, True, [H["kE"][h].k, TT_.k], [ph["x1"][h].k])
                    mm(kb, ph["x2"][h][:], TT_[:], vtm(h), True, True, [TT_.k, kv_.k], [ph["x2"][h].k])
                for h in HS:
                    cp(kb, "act", H["wT"][h][:], ph["x1"][h][:], [ph["x1"][h].k], [H["wT"][h].k])
                    ts(kb, "dve", H["ub"][h][:], ph["x2"][h][:], b4[:, h:h + 1], ALU.mult, [ph["x2"][h].k, gb_.k], [H["ub"][h].k])
                if GDBG.get("gstage", 99) < 6:
                    continue
                o_ = ot[i2]
                for c in ((0, 1) if dr == 0 else (1, 0)):
                    r = slice(c * 64, (c + 1) * 64)
                    for h in HS:
                        sk = Sst.k + str(h)
                        mm(kb, pws[h][r, :], H["wT"][h][:, r], Sst[:, h, :], True, True, [H["wT"][h].k, sk], [pws[h].k])
                    for h in HS:
                        stt(kb, "dve", H["vn"][h][r, :], pws[h][r, :], nb_[r, h:h + 1], H["ub"][h][r, :], ALU.mult, ALU.add,
                            [pws[h].k, nb_.k, H["ub"][h].k], [H["vn"][h].k])
                    for h in HS:
                        sk = Sst.k + str(h)
                        mm(kb, pob[h][r, :], H["qdT"][h][:, r], Sst[:, h, :], True, False, [H["qdT"][h].k, sk], [pob[h].k])
                        mm(kb, pob[h][r, :], H["QKm"][h][r, r], H["vn"][h][r, :], False, True, [H["QKm"][h].k, H["vn"][h].k], [pob[h].k])
                        mm(kb, psb[h][:], H["kdec"][h][r, :], H["vn"][h][r, :], True, True, [H["kdec"][h].k, H["vn"][h].k], [psb[h].k])
                    for h in HS:
                        sk = Sst.k + str(h)
                        stt(kb, "dve", Sst[:, h, :], Sst[:, h, :], GL_[:, h * 2 + c:h * 2 + c + 1], psb[h][:], ALU.mult, ALU.add,
                            [sk, GL_.k, psb[h].k], [sk])
                        if dr == 0:
                            cp(kb, "act", o_[r, h * 128:(h + 1) * 128], pob[h][r, :], [pob[h].k], [o_.k])
                        else:
                            tt(kb, "dve", o_[r, h * 128:(h + 1) * 128], pob[h][r, :], oa_[r, h * 128:(h + 1) * 128], ALU.add,
                               [pob[h].k, oa_.k], [o_.k])
                if dr == 0:
                    kb.dma("pool", "st_" + o_.k, oA_s[tok, :], o_[:], reads=[o_.k], writes=["oAdst"])
                else:
                    if dbg_oB is not None:
                        kb.dma("pool", "st_dbgoB", dbg_oB[tok, :], o_[:], reads=[o_.k], writes=["dbgoB"])
                    ss_ = ssq[i2]
                    yb_ = ybt[i2]
                    kb.op("dve", lambda e, o=ss_[:]: e.memset(o, 0.0), writes=[ss_.k])
                    for h in HS:
                        act(kb, junk[h % 2][:], o_[:, h * 128:(h + 1) * 128], AF.Square, [o_.k], [junk[h % 2].k, ss_.k], accum_out=ss_[:, h:h + 1])
                    act(kb, ss_[:], ss_[:], AF.Sqrt, [ss_.k, epsT.k], [ss_.k], bias=epsT[:, 0:1], scale=1.0 / 128.0)
                    kb.op("dve", lambda e, o=ss_[:]: e.reciprocal(out=o, in_=o), reads=[ss_.k], writes=[ss_.k])
                    for h in HS:
                        stt(kb, "dve", yb_[:, h * 128:(h + 1) * 128], o_[:, h * 128:(h + 1) * 128], ss_[:, h:h + 1], on_bc[:], ALU.mult, ALU.mult,
                            [o_.k, ss_.k, on_bc.k], [yb_.k])
                    tt(kb, "dve", yb_[:], yb_[:], zs_[:], ALU.mult, [yb_.k, zs_.k], [yb_.k])
                    if dbg_yb is not None:
                        kb.dma("pool", "st_dbgyb", dbg_yb[tok, :], yb_[:], reads=[yb_.k], writes=["dbgyb"])
                    yT_ = ybT[i2]
                    for h in HS:
                        tr(kb, ph["gr"][h][:], yb_[:, h * 128:(h + 1) * 128], ident, [yb_.k, CK], [ph["gr"][h].k])
                    for h in HS:
                        cp(kb, ("act", "dve")[h % 2], yT_[:, h, :], ph["gr"][h][:], [ph["gr"][h].k], [yT_.k])
                    kb.dma("pool", "st_" + yT_.k, ybT_s[:, :, tok], yT_[:], reads=[yT_.k], writes=["ybTdst"])
            Pg.close()
        if stop_after == "G":
            break

        if stage == "T":
            Pt = Pool(kb, "T%d" % l)
            TSK = GDBG.get("tskip", "")
            kTa = Pt.sb("kTa", [128, 4, TOK], BF16)
            Va = Pt.sb("Va", [128, NT, 4, VP], BF16)
            if "kta" not in TSK:
                for h in range(4):
                    for c0 in range(0, TOK, 1088):
                        kb.dma("sp", "kTa%d" % h, kTa[:, h, c0:c0 + 1088], kT_s[:, h, c0:c0 + 1088], reads=["qkdst1"], writes=[kTa.k + str(h)])
            if "va" not in TSK:
                for g in range(0, NT, 2):
                    kb.dma("sp", "Va%d" % (g % 4), Va[:, g:g + 2, :, :], V_s[g * 128:(g + 2) * 128, :, :].rearrange("(t p) h e -> p t h e", p=128),
                           reads=["Vdst"], writes=[Va.k + str(g)])
            KTK = [kTa.k + str(h) for h in range(4)]
            lamc = Pt.sb("lamc", [128, 2])
            kb.dma("sp", "lamc", lamc[:], lam_in, writes=[lamc.k])
            lv = Pt.sb("lv", [128, 4, 64])
            lp = Pt.sb("lp", [128, 2, 64])
            ls = Pt.sb("ls", [128, 2])
            nlam = Pt.sb("nlam", [128, 1])
            if "lam" not in TSK:
                for i in range(4):
                    kb.dma("sp", "lv%d" % i, lv[:, i, :], lamv[l][i:i + 1, :].partition_broadcast(128), writes=[lv.k + str(i)])
                tt(kb, "dve", lp[:, 0, :], lv[:, 0, :], lv[:, 1, :], ALU.mult, [lv.k + "0", lv.k + "1"], [lp.k])
                tt(kb, "dve", lp[:, 1, :], lv[:, 2, :], lv[:, 3, :], ALU.mult, [lv.k + "2", lv.k + "3"], [lp.k])
                kb.op("dve", lambda e: e.tensor_reduce(out=ls[:], in_=lp[:], axis=AX.X, op=ALU.add), reads=[lp.k], writes=[ls.k])
                act(kb, ls[:], ls[:], AF.Exp, [ls.k], [ls.k])
                tt(kb, "dve", nlam[:], ls[:, 1:2], ls[:, 0:1], ALU.subtract, [ls.k], [nlam.k])
                ts(kb, "dve", nlam[:], nlam[:], lamc[:, 0:1], ALU.add, [nlam.k, lamc.k], [nlam.k])
            sg = Pt.sb("sg", [128, 128])
            if "sg" not in TSK:
                kb.dma("sp", "sg", sg[:], sublng[l:l + 1, :].partition_broadcast(128), writes=[sg.k])
                ts(kb, "dve", sg[:], sg[:], lamc[:, 1:2], ALU.mult, [sg.k, lamc.k], [sg.k])
            qb = [[Pt.sb("qb%d_%d" % (i, n_), [128, 4, 512], BF16) for n_ in range(2)] for i in range(2)]
            for i in range(2):
                for n_ in range(2):
                    if "qbz" not in TSK:
                        kb.op("dve", lambda e, t_=qb[i][n_]: e.memset(t_[:], 0.0), writes=[qb[i][n_].k])
            pT = [Pt.sb("pT%d" % i, [128, 512], BF16) for i in range(3)]
            yat = [Pt.sb("yat%d" % i, [128, 512]) for i in range(4)]
            yaT = [Pt.sb("yaT%d" % i, [128, 4, 512], BF16) for i in range(2)]
            t0s = [Pt.sb("t0s%d" % i, [128, 128]) for i in range(2)]
            dd = [Pt.sb("dd%d" % i, [128, 128]) for i in range(2)]
            rr = [Pt.sb("rr%d" % i, [128, 4]) for i in range(2)]
            jk = [Pt.sb("jk%d" % i, [128, 128]) for i in range(2)]
            pst = [Pt.ps("pst%d" % i, [128, 512]) for i in range(2)]
            pacc = [[Pt.ps("pacc%d_%d" % (n_, i), [128, 2, 256]) for i in range(2)] for n_ in range(2)]
            ptr_ = Pt.ps("ptrT", [128, 512])
            ci = 0
            for bi, (t0, bs, isc) in enumerate(BLOCKS_T):
                ntl = bs // 128
                keyt = [32, 33] if isc else list(range(NT))
                qz = qb[bi % 2]
                for n_ in range(2):
                    rws = slice(n_ * 64, (n_ + 1) * 64)
                    kb.dma("sp", qz[n_].k, qz[n_][rws, :, 0:bs], qT_s[rws, :, t0:t0 + bs], reads=["qkdst0"], writes=[qz[n_].k])
                for h in range(4):
                    for n_ in range(2):
                        rows = slice(n_ * 64, (n_ + 1) * 64)
                        for ki, kt in enumerate(keyt):
                            ps = pst[ci % 2]
                            p_ = pT[ci % 3]
                            ci += 1
                            mm(kb, ps[:, 0:bs], kTa[:, h, kt * 128:(kt + 1) * 128], qz[n_][:, h, 0:bs], True, True, [KTK[h], qz[n_].k], [ps.k])
                            if GDBG.get("tstage", 99) < 0:
                                continue
                            act(kb, p_[:, 0:bs], ps[:, 0:bs], AF.Exp, [ps.k], [p_.k], scale=0.125)
                            if GDBG.get("tstage", 99) < 1:
                                continue
                            for q in range(ntl):
                                pa = pacc[n_][q // 2]
                                mm(kb, pa[:, q % 2, 0:129], p_[:, q * 128:(q + 1) * 128], Va[:, kt, h, 0:129], ki == 0 and q % 2 == 0, ki == len(keyt) - 1,
                                   [p_.k, Va.k + str(kt - kt % 2)], [pa.k], sgc=True)
                    if GDBG.get("tstage", 99) < 2:
                        continue
                    for q in range(ntl):
                        o0 = pacc[0][q // 2]
                        o1 = pacc[1][q // 2]
                        k0, k1 = o0.k, o1.k
                        r_ = rr[q % 2]
                        kb.op("dve", lambda e, o=r_[:, 0:1], i=o0[:, q % 2, 128:129]: e.reciprocal(out=o, in_=i), reads=[k0], writes=[r_.k])
                        kb.op("dve", lambda e, o=r_[:, 1:2], i=o1[:, q % 2, 128:129]: e.reciprocal(out=o, in_=i), reads=[k1], writes=[r_.k])
                        tt(kb, "dve", r_[:, 1:2], r_[:, 1:2], nlam[:], ALU.mult, [r_.k, nlam.k], [r_.k])
                        t_ = t0s[q % 2]
                        act(kb, t_[:], o0[:, q % 2, 0:128], AF.Copy, [k0, r_.k], [t_.k], scale=r_[:, 0:1])
                        d_ = dd[q % 2]
                        stt(kb, "dve", d_[:], o1[:, q % 2, 0:128], r_[:, 1:2], t_[:], ALU.mult, ALU.add, [k1, r_.k, t_.k], [d_.k])
                        kb.op("dve", lambda e, o=r_[:, 2:3]: e.memset(o, 0.0), writes=[r_.k])
                        act(kb, jk[q % 2][:], d_[:], AF.Square, [d_.k], [jk[q % 2].k, r_.k], accum_out=r_[:, 2:3])
                        act(kb, r_[:, 2:3], r_[:, 2:3], AF.Sqrt, [r_.k, epsT.k], [r_.k], bias=epsT[:, 0:1], scale=1.0 / 128.0)
                        kb.op("dve", lambda e, o=r_[:, 2:3]: e.reciprocal(out=o, in_=o), reads=[r_.k], writes=[r_.k])
                        ya_ = yat[q]
                        stt(kb, "dve", ya_[:, h * 128:(h + 1) * 128], d_[:], r_[:, 2:3], sg[:], ALU.mult, ALU.mult, [d_.k, r_.k, sg.k], [ya_.k])
                yT_ = yaT[bi % 2]
                if GDBG.get("tstage", 99) < 3:
                    continue
                for q in range(ntl):
                    if dbg_ya is not None:
                        kb.dma("pool", "st_dbgya", dbg_ya[t0 + q * 128:t0 + (q + 1) * 128, :], yat[q][:], reads=[yat[q].k], writes=["dbgya"])
                    for h in range(4):
                        tr(kb, ptr_[:, h * 128:(h + 1) * 128], yat[q][:, h * 128:(h + 1) * 128], ident, [yat[q].k, CK], [ptr_.k])
                    cp(kb, ("act", "dve")[q % 2], yT_[:, :, q * 128:(q + 1) * 128], ptr_[:].rearrange("p (h e) -> p h e", h=4), [ptr_.k], [yT_.k])
                kb.dma("pool", "st_" + yT_.k, yaT_s[:, :, t0:t0 + bs], yT_[:, :, 0:bs], reads=[yT_.k], writes=["yaTdst"])
            Pt.close()
        if stop_after == "T":
            break

        def layer_norm_out(Pm, ln, z_, gi, bi_, x_out, eng_last="pool"):
            st_, mv_, sd_ = ln
            zz = z_[:].rearrange("p (c f) -> p c f", c=2)
            for c_ in range(2):
                kb.op("dve", lambda e, o=st_[:, c_, :], i=zz[:, c_, :]: e.bn_stats(out=o, in_=i), reads=[z_.k], writes=[st_.k])
            kb.op("dve", lambda e: e.bn_aggr(out=mv_[:], in_=st_[:]), reads=[st_.k], writes=[mv_.k])
            act(kb, sd_[:, 0:1], mv_[:, 1:2], AF.Sqrt, [mv_.k, epsT.k], [sd_.k], bias=epsT[:, 0:1], scale=1.0)
            kb.op("dve", lambda e: e.reciprocal(out=sd_[:, 0:1], in_=sd_[:, 0:1]), reads=[sd_.k], writes=[sd_.k])
            stt(kb, "dve", sd_[:, 1:2], mv_[:, 0:1], -1.0, sd_[:, 0:1], ALU.mult, ALU.mult, [mv_.k, sd_.k], [sd_.k])
            act(kb, z_[:], z_[:], AF.Identity, [z_.k, sd_.k], [z_.k], scale=sd_[:, 0:1], bias=sd_[:, 1:2])
            tt(kb, "dve", z_[:], z_[:], lnbc[:, gi, :], ALU.mult, [z_.k, LNK[gi]], [z_.k])
            tt(kb, eng_last, x_out[:], z_[:], lnbc[:, bi_, :], ALU.add, [z_.k, LNK[bi_]], [x_out.k])

        if stage == "M":
            Pm = Pool(kb, "M%d" % l)
            wstage = [Pm.sb("wstg%d" % i, [128, 8, 256]) for i in range(2)]
            wcnt = [0]
            Wpa = Pm.sb("Wpa", [128, 4, D], BF16)
            Wpb = Pm.sb("Wpb", [128, 4, D], BF16)
            Wgt = Pm.sb("Wgt", [128, 8, 2048], BF16)
            Wo = Pm.sb("Wo", [128, 8, D], BF16)
            load_w_bf16(kb, C, Pm, Wpa, 0, w_pa[l], D, 4, wstage, wcnt)
            load_w_bf16(kb, C, Pm, Wpb, 0, w_pb[l], D, 4, wstage, wcnt)
            load_w_bf16(kb, C, Pm, Wgt, 0, w_gate, 2048, 8, wstage, wcnt)
            load_w_bf16(kb, C, Pm, Wo, 0, w_o[l], D, 8, wstage, wcnt)
            hb = [Pm.sb("hb%d" % i, [128, 8, 512], BF16) for i in range(2)]
            yab = [Pm.sb("yab%d" % i, [128, 4, 512], BF16) for i in range(2)]
            ybb = [Pm.sb("ybb%d" % i, [128, 4, 512], BF16) for i in range(2)]
            mT = [Pm.sb("mT%d" % i, [128, 8, 512], BF16) for i in range(2)]
            sga = [Pm.sb("sga%d" % i, [128, 512]) for i in range(2)]
            sgb = [Pm.sb("sgb%d" % i, [128, 512]) for i in range(2)]
            xt = [Pm.sb("xt%d" % i, [128, D]) for i in range(3)]
            zt_ = [Pm.sb("zt%d" % i, [128, D]) for i in range(3)]
            lnst = [(Pm.sb("st%d" % i, [128, 2, 6]), Pm.sb("mv%d" % i, [128, 2]), Pm.sb("sd%d" % i, [128, 2])) for i in range(2)]
            ppa = Pm.ps("ppa", [128, 512])
            ppb = Pm.ps("ppb", [128, 512])
            pga = Pm.ps("pga", [128, 512])
            pgb = Pm.ps("pgb", [128, 512])
            py = [Pm.ps("py%d" % i, [128, 512]) for i in range(4)]
            tc = 0
            for bi, (t0, bs, isc) in enumerate(BLOCKS_E):
                ntl = bs // 128
                jj = 1 if isc else 0
                h_, ya_, yb_, m_ = hb[bi % 2], yab[bi % 2], ybb[bi % 2], mT[bi % 2]
                kb.dma("sp", h_.k, h_[:, :, 0:bs], hT_s[:, :, t0:t0 + bs], reads=["hTdst"], writes=[h_.k])
                kb.dma("sp", ya_.k, ya_[:, :, 0:bs], yaT_s[:, :, t0:t0 + bs], reads=["yaTdst"], writes=[ya_.k])
                kb.dma("sp", yb_.k, yb_[:, :, 0:bs], ybT_s[:, :, t0:t0 + bs], reads=["ybTdst"], writes=[yb_.k])
                for fc in range(8):
                    cs = slice(fc * 128, (fc + 1) * 128)
                    for k in range(4):
                        mm(kb, ppa[:, 0:bs], Wpa[:, k, cs], ya_[:, k, 0:bs], k == 0, k == 3, [Wpa.k, ya_.k], [ppa.k])
                    for k in range(4):
                        mm(kb, ppb[:, 0:bs], Wpb[:, k, cs], yb_[:, k, 0:bs], k == 0, k == 3, [Wpb.k, yb_.k], [ppb.k])
                    for k in range(8):
                        mm(kb, pga[:, 0:bs], Wgt[:, k, cs], h_[:, k, 0:bs], k == 0, k == 7, [Wgt.k, h_.k], [pga.k])
                    for k in range(8):
                        mm(kb, pgb[:, 0:bs], Wgt[:, k, 1024 + fc * 128:1024 + (fc + 1) * 128], h_[:, k, 0:bs], k == 0, k == 7, [Wgt.k, h_.k], [pgb.k])
                    sa, sb2 = sga[fc % 2], sgb[fc % 2]
                    act(kb, sa[:, 0:bs], pga[:, 0:bs], AF.Sigmoid, [pga.k], [sa.k])
                    act(kb, sb2[:, 0:bs], pgb[:, 0:bs], AF.Sigmoid, [pgb.k], [sb2.k])
                    tt(kb, "dve", sa[:, 0:bs], sa[:, 0:bs], ppa[:, 0:bs], ALU.mult, [sa.k, ppa.k], [sa.k])
                    tt(kb, "dve", sb2[:, 0:bs], sb2[:, 0:bs], ppb[:, 0:bs], ALU.mult, [sb2.k, ppb.k], [sb2.k])
                    tt(kb, "pool", m_[:, fc, 0:bs], sa[:, 0:bs], sb2[:, 0:bs], ALU.add, [sa.k, sb2.k], [m_.k])
                for q in range(ntl):
                    tok = slice(t0 + q * 128, t0 + (q + 1) * 128)
                    x_ = xt[tc % 3]
                    z_ = zt_[tc % 3]
                    kb.dma("sp", x_.k, x_[:], xs_cur[tok, :], writes=[x_.k])
                    for half in range(2):
                        ps = py[(tc * 2 + half) % 4]
                        for k in range(8):
                            mm(kb, ps[:], m_[:, k, q * 128:(q + 1) * 128], Wo[:, k, half * 512:(half + 1) * 512], k == 0, k == 7, [m_.k, Wo.k], [ps.k])
                        if dbg_ymix is not None:
                            cp(kb, "act", z_[:, half * 512:(half + 1) * 512], ps[:], [ps.k], [z_.k])
                        else:
                            tt(kb, "dve", z_[:, half * 512:(half + 1) * 512], ps[:], gbc[("m2", jj)][:, half * 512:(half + 1) * 512], ALU.mult,
                               [ps.k, gbc[("m2", jj)].k], [z_.k])
                    if dbg_ymix is not None:
                        kb.dma("pool", "st_dbgymix", dbg_ymix[tok, :], z_[:], reads=[z_.k], writes=["dbgymix"])
                        tt(kb, "dve", z_[:], z_[:], gbc[("m2", jj)][:], ALU.mult, [z_.k, gbc[("m2", jj)].k], [z_.k])
                    stt(kb, "dve", z_[:], x_[:], ALPHA, z_[:], ALU.mult, ALU.add, [x_.k, z_.k], [z_.k])
                    layer_norm_out(Pm, lnst[tc % 2], z_, 0, 1, x_)
                    kb.dma("pool", "st_" + x_.k, x1_s[tok, :], x_[:], reads=[x_.k], writes=["x1dst"])
                    tc += 1
            Pm.close()
        if stop_after == "M":
            break

        class _Sh:
            pass
        if stage == "E":
            make_hT("E1_%d" % l, x1_s, sc4, _ShiftView(modT[l], 24), hT_s, want_router=True, blocks=BLOCKS_E)

            Pe = Pool(kb, "E2_%d" % l)
            wst1 = [Pe.sb("wst1_%d" % i, [128, 8, 256]) for i in range(2)]
            W1 = [Pe.sb("W1_%d" % i, [128, 8, 512], BF16) for i in range(2)]
            W3 = [Pe.sb("W3_%d" % i, [128, 8, 512], BF16) for i in range(2)]
            W2 = [Pe.sb("W2_%d" % i, [128, 4, D], BF16) for i in range(2)]
            passes = [(0, 9), (9, 8)]
            if GDBG.get("e_passes") is not None:
                passes = passes[:GDBG["e_passes"]]
            hp = Pe.sb("hp", [128, 8, 12 * 128], BF16)
            wtp = Pe.sb("wtp", [16, 12 * 128])
            yacc = Pe.sb("yacc", [128, 12, D])
            aT = [Pe.sb("aT%d" % i, [128, 4, 512], BF16) for i in range(2)]
            s1 = [Pe.sb("s1_%d" % i, [128, 512]) for i in range(2)]
            wb = [Pe.sb("wb%d" % i, [128, 512]) for i in range(2)]
            xt = [Pe.sb("xt%d" % i, [128, D]) for i in range(2)]
            lnst = [(Pe.sb("st%d" % i, [128, 2, 6]), Pe.sb("mv%d" % i, [128, 2]), Pe.sb("sd%d" % i, [128, 2])) for i in range(2)]
            pw = [Pe.ps("pw%d" % i, [128, 512]) for i in range(1)]
            p1 = [Pe.ps("p1_%d" % i, [128, 512]) for i in range(2)]
            p3 = [Pe.ps("p3_%d" % i, [128, 512]) for i in range(2)]
            py = [Pe.ps("py%d" % i, [128, 512]) for i in range(3)]
            wc = [0]
            ec = 0
            for (tile0, ntile) in passes:
                ntok = ntile * 128
                tk0 = tile0 * 128
                kb.dma("sp", "hp", hp[:, :, 0:ntok], hT_s[:, :, tk0:tk0 + ntok], reads=["hTdst"], writes=[hp.k])
                kb.dma("sp", "wtp", wtp[:, 0:ntok], wtT_s[:, tk0:tk0 + ntok], reads=["wtTdst"], writes=[wtp.k])
                subs = []
                o_ = 0
                while o_ < ntok:
                    w_ = min(512, ntok - o_)
                    subs.append((o_, w_))
                    o_ += w_
                for e_ in range(GDBG.get("e_nexp", 16)):
                    W1_, W3_, W2_ = W1[ec % 2], W3[ec % 2], W2[ec % 2]
                    ec += 1
                    load_w_bf16(kb, C, Pe, W1_, 0, w_e1[l][e_], 512, 8, wst1, wc)
                    load_w_bf16(kb, C, Pe, W3_, 0, w_e3[l][e_], 512, 8, wst1, wc)
                    load_w_bf16(kb, C, Pe, W2_, 0, w_e2[l][e_], D, 4, wst1, wc)
                    for si, (o_, w_) in enumerate(subs):
                        a_ = aT[si % 2]
                        wb_ = wb[si % 2]
                        mm(kb, pw[0][:, 0:w_], sel16[:, e_, :], wtp[:, o_:o_ + w_], True, True, [sel16.k, wtp.k], [pw[0].k])
                        cp(kb, "act", wb_[:, 0:w_], pw[0][:, 0:w_], [pw[0].k], [wb_.k])
                        for f in range(4):
                            fs = slice(f * 128, (f + 1) * 128)
                            pp1, pp3 = p1[f % 2], p3[f % 2]
                            for k in range(8):
                                mm(kb, pp1[:, 0:w_], W1_[:, k, fs], hp[:, k, o_:o_ + w_], k == 0, k == 7, [W1_.k, hp.k], [pp1.k])
                            for k in range(8):
                                mm(kb, pp3[:, 0:w_], W3_[:, k, fs], hp[:, k, o_:o_ + w_], k == 0, k == 7, [W3_.k, hp.k], [pp3.k])
                            s_ = s1[f % 2]
                            act(kb, s_[:, 0:w_], pp1[:, 0:w_], AF.Silu, [pp1.k], [s_.k])
                            tt(kb, "dve", s_[:, 0:w_], s_[:, 0:w_], pp3[:, 0:w_], ALU.mult, [s_.k, pp3.k], [s_.k])
                            tt(kb, "pool", a_[:, f, 0:w_], s_[:, 0:w_], wb_[:, 0:w_], ALU.mult, [s_.k, wb_.k], [a_.k])
                        for q in range(w_ // 128):
                            tl = (o_ // 128) + q
                            for half in range(2):
                                ps = py[(q * 2 + half) % 3]
                                for f in range(4):
                                    mm(kb, ps[:], a_[:, f, q * 128:(q + 1) * 128], W2_[:, f, half * 512:(half + 1) * 512], f == 0, f == 3,
                                       [a_.k, W2_.k], [ps.k])
                                yk = yacc.k + str(tl)
                                if e_ == 0:
                                    cp(kb, "act", yacc[:, tl, half * 512:(half + 1) * 512], ps[:], [ps.k], [yk])
                                else:
                                    tt(kb, "dve", yacc[:, tl, half * 512:(half + 1) * 512], ps[:], yacc[:, tl, half * 512:(half + 1) * 512], ALU.add,
                                       [ps.k, yk], [yk])
                for tl in range(ntile):
                    ti = tile0 + tl
                    tok = slice(ti * 128, (ti + 1) * 128)
                    jj = 1 if ti >= 16 else 0
                    x_ = xt[tl % 2]
                    yk = yacc.k + str(tl)
                    kb.dma("sp", x_.k, x_[:], x1_s[tok, :], reads=["x1dst"], writes=[x_.k])
                    if dbg_ymoe is not None:
                        kb.dma("pool", "st_dbgymoe", dbg_ymoe[tok, :], yacc[:, tl, :], reads=[yk], writes=["dbgymoe"])
                    tt(kb, "dve", yacc[:, tl, :], yacc[:, tl, :], gbc[("m5", jj)][:], ALU.mult, [yk, gbc[("m5", jj)].k], [yk])
                    stt(kb, "dve", yacc[:, tl, :], x_[:], ALPHA, yacc[:, tl, :], ALU.mult, ALU.add, [x_.k, yk], [yk])
                    zt = T(yacc.t[:, tl, :], yk)
                    layer_norm_out(Pe, lnst[tl % 2], zt, 2, 3, x_)
                    kb.dma("pool", "st_" + x_.k, xs_next[tok, :], x_[:], reads=[x_.k], writes=["xsdst"])
            Pe.close()
        LP.close()
        xs_cur = xs_next
    kb.finish()
    return nc


class _ShiftView:
    def __init__(self, t, c0):
        self.t = t
        self.c0 = c0
        self.k = t.k

    def __getitem__(self, idx):
        p, k, j = idx
        return self.t[p, self.c0 + k, j]


def _consts():
    i = np.arange(128)
    same = (i[:, None] // 64) == (i[None, :] // 64)
    cm = np.zeros((128, 10, 128), np.float32)
    cm[:, 0] = np.eye(128)
    cm[:, 1] = 1.0
    inclA = same & (i[None, :] >= i[:, None])
    inclB = same & (i[None, :] <= i[:, None])
    cm[:, 2] = inclA
    cm[:, 3] = inclB
    cm[:, 4] = np.where(inclA, 0.0, NEGBIG)
    cm[:, 5] = np.where(inclB, 0.0, NEGBIG)
    cm[:, 6] = inclA & ~np.eye(128, dtype=bool)
    cm[:, 7] = inclB & ~np.eye(128, dtype=bool)
    cm[:, 8, 0] = (i < 64)
    cm[:, 8, 1] = (i >= 64)
    cm[:, 9] = same
    t = np.arange(S)
    row = (t // 64).astype(np.float32)
    col = (t % 64).astype(np.float32)
    inv = (10000.0 ** (-np.arange(16, dtype=np.float32) / 16)).astype(np.float32)
    rt = np.zeros((2, 128, S), np.float32)
    for p in range(128):
        d = p % 64
        a, half, f = d // 32, (d % 32) // 16, d % 16
        ang = ((row if a == 0 else col) * inv[f]).astype(np.float32)
        rt[0, p] = np.cos(ang)
        rt[1, p] = np.sin(ang) * (-1.0 if half == 0 else 1.0)
    sel = np.zeros((16, 16, 128), np.float32)
    for e in range(16):
        sel[e, e, :] = 1.0
    return cm, rt, sel


def _layer_inputs(inp, l):
    w_in = inp["w_in"][l]
    d = np.arange(64)
    partner = np.where((d % 32) < 16, d + 16, d - 16)
    perm = (np.arange(512) // 64) * 64 + partner[np.arange(512) % 64]
    cols = np.concatenate([
        np.arange(0, 512), perm, 512 + np.arange(512), 512 + perm, 1024 + np.arange(512),
        1536 + np.arange(1536), 3072 + np.arange(512), 3584 + np.arange(16)])
    lam_init = 0.8 - 0.6 * math.exp(-0.3 * l)
    c = np.ascontiguousarray
    return {
        "mod_w": c(inp["mod_w"][l:l + 1]),
        "mod_bT": c(inp["mod_b"][l:l + 1].reshape(1, 48, 128).transpose(0, 2, 1)),
        "w_in": c(w_in[:, cols])[None],
        "w_gate": c(w_in[:, 3600:3600 + 2048]),
        "cw": c(inp["conv_w"][l:l + 1].reshape(1, 5, 12, 128).transpose(0, 3, 2, 1)),
        "lamv": c(np.stack([inp["lam_q1"][l], inp["lam_k1"][l], inp["lam_q2"][l], inp["lam_k2"][l]], 0))[None],
        "lam_in": c(np.tile(np.array([[-lam_init, 1.0 - lam_init]], np.float32), (128, 1))),
        "sublng": c(inp["subln_g"][l:l + 1]),
        "onormg": c(inp["onorm_g"][l:l + 1]),
        "alog": c(inp["a_log"][l:l + 1].reshape(1, 8)),
        "dtb": c(inp["dt_bias"][l:l + 1].reshape(1, 8)),
        "w_pa": c(inp["w_pa"][l:l + 1]),
        "w_pb": c(inp["w_pb"][l:l + 1]),
        "w_o": c(inp["w_o"][l:l + 1]),
        "lnp": c(np.stack([inp["ln1_g"][l], inp["ln1_b"][l], inp["ln2_g"][l], inp["ln2_b"][l]], 0))[None],
        "router_w": c(inp["router_w"]),
        "router_b": c(inp["router_b"].reshape(1, 16)),
        "w_e1": c(inp["w_exp1"][l:l + 1]),
        "w_e3": c(inp["w_exp3"][l:l + 1]),
        "w_e2": c(inp["w_exp2"][l:l + 1]),
    }


_PROGS = {}


def _run_stage(stage, shared, per_core, dbg=()):
    key = (stage, tuple(dbg))
    nc = build_program(stage, dbg)
    in_maps = []
    for pc in per_core:
        m = {}
        for name in STAGE_IN[stage]:
            m[name] = pc[name] if name in pc else shared[name]
        in_maps.append(m)
    res = run_bass_kernel_spmd(nc, in_maps, core_ids=list(range(len(per_core))))
    return res.results


def run_layers(inp, layers=range(DEPTH), cores=(0, 1, 2, 3), x_override=None, dbg_hook=None):
    inp = {k: np.asarray(v, dtype=np.float32) for k, v in inp.items()}
    cm, rt, sel = _consts()
    state = []
    for b in cores:
        st = {"x_in": np.ascontiguousarray(np.concatenate([inp["x"][b], inp["ctx"][b]], 0)),
              "cvec": np.ascontiguousarray(np.stack([inp["c"][b].reshape(8, 128).T, inp["c_ctx"].reshape(8, 128).T], -1))}
        state.append(st)
    H = S // 2
    for l in layers:
        sh = _layer_inputs(inp, l)
        sh.update({"cmat": cm, "ropet": rt, "sel16": sel})
        r = _run_stage("A", sh, state)
        for st, o in zip(state, r):
            for k in STAGE_OUT["A"]:
                st[k if k != "modT_o" else "modT_i"] = np.asarray(o[k])
        r = _run_stage("G0", sh, state)
        for st, o in zip(state, r):
            st["oA_s"] = np.asarray(o["oA_s"])
        r = _run_stage("G1", sh, state)
        for st, o in zip(state, r):
            st["ybT_s"] = np.asarray(o["ybT_s"])
        pcs = []
        for st in state:
            q = st["qT_s"]
            for hh in range(2):
                pc = dict(st)
                pc["qT_s"] = np.ascontiguousarray(np.concatenate([q[:, :, hh * H:(hh + 1) * H], q[:, :, S:]], 2))
                pcs.append(pc)
        r = _run_stage("T", sh, pcs)
        for i, st in enumerate(state):
            y0, y1 = np.asarray(r[2 * i]["yaT_s"]), np.asarray(r[2 * i + 1]["yaT_s"])
            st["yaT_s"] = np.ascontiguousarray(np.concatenate([y0[:, :, :H], y1[:, :, :H], y0[:, :, H:]], 2))
        pcs = []
        for st in state:
            for hh in range(2):
                pc = dict(st)
                rows = np.r_[hh * H:(hh + 1) * H, S + hh * 128:S + (hh + 1) * 128]
                pc["x_in"] = np.ascontiguousarray(st["x_in"][rows])
                for k in ("hT_s", "yaT_s", "ybT_s"):
                    pc[k] = np.ascontiguousarray(st[k][:, :, rows])
                pcs.append(pc)
        r = _run_stage("M", sh, pcs)
        for pc, o in zip(pcs, r):
            pc["x1_s"] = np.asarray(o["x1_s"])
        r = _run_stage("E", sh, pcs)
        for i, st in enumerate(state):
            x0, x1_ = np.asarray(r[2 * i]["xsA"], dtype=np.float32), np.asarray(r[2 * i + 1]["xsA"], dtype=np.float32)
            st["x_in"] = np.ascontiguousarray(np.concatenate([x0[:H], x1_[:H], x0[H:], x1_[H:]], 0))
        if dbg_hook is not None:
            dbg_hook(l, state)
    return state


def kernel(**inputs):
    state = run_layers(inputs)
    return np.stack([np.ascontiguousarray(st["x_in"][:S], dtype=np.float32) for st in state], 0)
```
